# Optimizing a Trainium2 kernel written in Bass

```python
import math
import jax, jax.numpy as jnp
from jax import lax
import numpy as np

D_MODEL = 2048
BATCH = 2
SEQ = 16384
DEPTH = 1

SB_HEADS = 8
SB_HEAD_DIM = D_MODEL // 16
SB_WIDTH = SB_HEADS * SB_HEAD_DIM
Q_BLOCK = 128
GM_GROUPS = 8
GM_GROUP_DIM = D_MODEL // 16
GM_WIDTH = GM_GROUPS * GM_GROUP_DIM
GM_CHUNK = 128
IN_WIDTH = 3 * SB_WIDTH + 2 * GM_WIDTH + 2 * D_MODEL
N_MEM = 256
X_HEADS = 4
X_HEAD_DIM = D_MODEL // 16
X_WIDTH = X_HEADS * X_HEAD_DIM
N_GROUPS = 4
EXPERTS_PER_GROUP = 8
N_EXPERTS = N_GROUPS * EXPERTS_PER_GROUP
TOP_K = 2
D_EXPERT = D_MODEL // 2
MOE_BLOCK = 128
EPS = 1e-6

kernel_name = "hybrid_stickbreak_gmlp_hmoe"


def rmsnorm(x, g):
    xf = x.astype(jnp.float32)
    y = xf * lax.rsqrt(jnp.mean(xf * xf, axis=-1, keepdims=True) + EPS)
    return (y * g.astype(jnp.float32)).astype(x.dtype)


def stick_breaking_attention(q, k, v):
    B, S, H, Dh = q.shape
    nb = S // Q_BLOCK
    scale = 1.0 / math.sqrt(Dh)
    kh = k.transpose(0, 2, 1, 3)
    vh = v.transpose(0, 2, 1, 3)
    qb = q.reshape(B, nb, Q_BLOCK, H, Dh).transpose(1, 0, 3, 2, 4)
    key_pos = jnp.arange(S)

    def one_block(args):
        q_i, i = args
        z = jnp.einsum('bhqd,bhkd->bhqk', q_i, kh, preferred_element_type=jnp.float32) * scale
        q_pos = i * Q_BLOCK + jnp.arange(Q_BLOCK)
        strict = key_pos[None, :] < q_pos[:, None]
        log_not = jnp.where(strict, jax.nn.log_sigmoid(-z), 0.0)
        between = lax.cumsum(log_not, axis=3, reverse=True) - log_not
        a = jnp.where(strict, jnp.exp(jax.nn.log_sigmoid(z) + between), 0.0)
        return jnp.einsum('bhqk,bhkd->bhqd', a.astype(vh.dtype), vh)

    o = lax.map(one_block, (qb, jnp.arange(nb)))
    return o.transpose(1, 0, 3, 2, 4).reshape(B, S, H * Dh)


def chunked_spatial_gating(u, v, g_norm, w_s, b_s):
    B, S, W = u.shape
    nc = S // GM_CHUNK
    v = rmsnorm(v, g_norm)
    vg = v.reshape(B, nc, GM_CHUNK, GM_GROUPS, GM_GROUP_DIM)
    mask = jnp.tril(jnp.ones((GM_CHUNK, GM_CHUNK), dtype=w_s.dtype))
    w = (w_s * mask[None]).astype(v.dtype)
    mixed = jnp.einsum('gts,bcsge->bctge', w, vg) + b_s.T.astype(v.dtype)[None, None, :, :, None]
    return u * mixed.reshape(B, S, W)


def memory_cross_attention(h, mem, w_xq, w_xkv, w_xo):
    B, S, _ = h.shape
    M = mem.shape[1]
    q = (h @ w_xq).reshape(B, S, X_HEADS, X_HEAD_DIM)
    k, v = jnp.split(mem @ w_xkv, 2, axis=-1)
    k = k.reshape(B, M, X_HEADS, X_HEAD_DIM)
    v = v.reshape(B, M, X_HEADS, X_HEAD_DIM)
    s = jnp.einsum('bqhd,bmhd->bhqm', q, k, preferred_element_type=jnp.float32) / math.sqrt(X_HEAD_DIM)
    p = jax.nn.softmax(s, axis=-1)
    o = jnp.einsum('bhqm,bmhd->bqhd', p.astype(v.dtype), v).reshape(B, S, X_WIDTH)
    return o @ w_xo


def hierarchical_moe(h, w_rg, b_rg, w_re, b_re, w_gate, w_up, w_down):
    B, S, D = h.shape
    T = B * S
    hf = h.reshape(T, D)
    g_logits = (hf @ w_rg).astype(jnp.float32) + b_rg.astype(jnp.float32)
    g_prob = jax.nn.softmax(g_logits, axis=-1)
    g_idx = jnp.argmax(g_logits, axis=-1).astype(jnp.int32)
    p_g = jnp.take_along_axis(g_prob, g_idx[:, None], axis=1)[:, 0]
    e_all = (hf @ w_re).astype(jnp.float32) + b_re.astype(jnp.float32)
    e_logits = jnp.take_along_axis(e_all.reshape(T, N_GROUPS, EXPERTS_PER_GROUP), g_idx[:, None, None], axis=1)[:, 0]
    top_v, top_i = lax.top_k(e_logits, TOP_K)
    weights = p_g[:, None] * jax.nn.softmax(top_v, axis=-1)
    expert = g_idx[:, None] * EXPERTS_PER_GROUP + top_i.astype(jnp.int32)
    n = T * TOP_K
    flat_e = expert.reshape(n)
    flat_tok = jnp.repeat(jnp.arange(T, dtype=jnp.int32), TOP_K)
    flat_w = weights.reshape(n)
    order = jnp.argsort(flat_e)
    sorted_e = flat_e[order]
    counts = jnp.bincount(flat_e, length=N_EXPERTS)
    padded = ((counts + MOE_BLOCK - 1) // MOE_BLOCK) * MOE_BLOCK
    starts = jnp.cumsum(counts) - counts
    pends = jnp.cumsum(padded)
    pstarts = pends - padded
    dest = pstarts[sorted_e] + jnp.arange(n) - starts[sorted_e]
    P = (-(-n // MOE_BLOCK) + N_EXPERTS) * MOE_BLOCK
    tok_buf = jnp.zeros((P,), jnp.int32).at[dest].set(flat_tok[order])
    w_buf = jnp.zeros((P,), jnp.float32).at[dest].set(flat_w[order])
    nblk = P // MOE_BLOCK
    blk_start = jnp.arange(nblk) * MOE_BLOCK
    blk_e = jnp.minimum(jnp.sum(pends[None, :] <= blk_start[:, None], axis=1), N_EXPERTS - 1)

    def run_block(args):
        tok, wt, e = args
        xb = hf[tok]
        y = (jax.nn.silu(xb @ w_gate[e]) * (xb @ w_up[e])) @ w_down[e]
        return y * wt[:, None].astype(y.dtype)

    ys = lax.map(run_block, (tok_buf.reshape(nblk, MOE_BLOCK), w_buf.reshape(nblk, MOE_BLOCK), blk_e))
    out = jnp.zeros((T, D), h.dtype).at[tok_buf].add(ys.reshape(P, D))
    return out.reshape(B, S, D)


def setup_inputs(seed: int = 0) -> dict:
    key = jax.random.key(seed)
    ks = jax.random.split(key, 24)
    f32 = jnp.float32
    L, D = DEPTH, D_MODEL

    def nrm(k, shape, fan_in):
        return jax.random.normal(k, shape, f32) * (fan_in ** -0.5)

    def gain(k, shape):
        return 1.0 + 0.02 * jax.random.normal(k, shape, f32)

    return {
        "x": jax.random.normal(ks[0], (BATCH, SEQ, D), f32),
        "mem": jax.random.normal(ks[1], (BATCH, N_MEM, D), f32),
        "g_mix": gain(ks[2], (L, D)),
        "w_in": nrm(ks[3], (L, D, IN_WIDTH), D),
        "g_gm": gain(ks[4], (L, GM_WIDTH)),
        "w_spatial": nrm(ks[5], (L, GM_GROUPS, GM_CHUNK, GM_CHUNK), GM_CHUNK),
        "b_spatial": 1.0 + 0.1 * jax.random.normal(ks[6], (L, GM_GROUPS, GM_CHUNK), f32),
        "w_branch_sb": nrm(ks[7], (L, SB_WIDTH, D), SB_WIDTH),
        "w_branch_gm": nrm(ks[8], (L, GM_WIDTH, D), GM_WIDTH),
        "w_out": nrm(ks[9], (L, D, D), D),
        "g_cross": gain(ks[10], (L, D)),
        "g_mem": gain(ks[11], (L, D)),
        "w_xq": nrm(ks[12], (L, D, X_WIDTH), D),
        "w_xkv": nrm(ks[13], (L, D, 2 * X_WIDTH), D),
        "w_xo": nrm(ks[14], (L, X_WIDTH, D), X_WIDTH),
        "g_ffn": gain(ks[15], (L, D)),
        "w_rg": nrm(ks[16], (L, D, N_GROUPS), D),
        "b_rg": 0.01 * jax.random.normal(ks[17], (L, N_GROUPS), f32),
        "w_re": nrm(ks[18], (L, D, N_EXPERTS), D),
        "b_re": 0.01 * jax.random.normal(ks[19], (L, N_EXPERTS), f32),
        "w_e_gate": nrm(ks[20], (L, N_EXPERTS, D, D_EXPERT), D),
        "w_e_up": nrm(ks[21], (L, N_EXPERTS, D, D_EXPERT), D),
        "w_e_down": nrm(ks[22], (L, N_EXPERTS, D_EXPERT, D), D_EXPERT),
        "g_final": gain(ks[23], (D,)),
    }


def reference(x, mem, g_mix, w_in, g_gm, w_spatial, b_spatial, w_branch_sb, w_branch_gm, w_out,
              g_cross, g_mem, w_xq, w_xkv, w_xo, g_ffn, w_rg, b_rg, w_re, b_re,
              w_e_gate, w_e_up, w_e_down, g_final):
    B, S, D = x.shape
    splits = [SB_WIDTH, 2 * SB_WIDTH, 3 * SB_WIDTH, 3 * SB_WIDTH + GM_WIDTH,
              3 * SB_WIDTH + 2 * GM_WIDTH, 3 * SB_WIDTH + 2 * GM_WIDTH + D_MODEL]
    for l in range(DEPTH):
        hn = rmsnorm(x, g_mix[l])
        proj = hn @ w_in[l]
        q, k, v, u_gm, v_gm, gate_sb, gate_gm = jnp.split(proj, splits, axis=-1)
        o_sb = stick_breaking_attention(q.reshape(B, S, SB_HEADS, SB_HEAD_DIM),
                                        k.reshape(B, S, SB_HEADS, SB_HEAD_DIM),
                                        v.reshape(B, S, SB_HEADS, SB_HEAD_DIM))
        o_gm = chunked_spatial_gating(jax.nn.gelu(u_gm), jax.nn.gelu(v_gm),
                                      g_gm[l], w_spatial[l], b_spatial[l])
        merged = (jax.nn.sigmoid(gate_sb) * (o_sb @ w_branch_sb[l])
                  + jax.nn.sigmoid(gate_gm) * (o_gm @ w_branch_gm[l]))
        x = x + merged @ w_out[l]
        x = x + memory_cross_attention(rmsnorm(x, g_cross[l]), rmsnorm(mem, g_mem[l]),
                                       w_xq[l], w_xkv[l], w_xo[l])
        x = x + hierarchical_moe(rmsnorm(x, g_ffn[l]), w_rg[l], b_rg[l], w_re[l], b_re[l],
                                 w_e_gate[l], w_e_up[l], w_e_down[l])
    return rmsnorm(x, g_final)
```

```python
import math
from contextlib import ExitStack

import numpy as np
import concourse.bass as bass
import concourse.mybir as mybir
from concourse.bass_utils import run_bass_kernel_spmd
from concourse.alu_op_type import AluOpType as ALU

AF = mybir.ActivationFunctionType
F32 = mybir.dt.float32
BF16 = mybir.dt.bfloat16
I32 = mybir.dt.int32
AX = mybir.AxisListType

D = 2048
NE = 32
DFF = 1024
NMEM = 256
EPS = 1e-6


class Buf:
    __slots__ = ("w", "r", "x")

    def __init__(self, x=False):
        self.w = None
        self.r = {}
        self.x = x


class TT:
    __slots__ = ("t", "b")

    def __init__(self, t):
        self.t = t
        self.b = Buf()


class EngState:
    def __init__(self, name, eng):
        self.name = name
        self.eng = eng
        self.sem = None
        self.count = 0
        self.known = {}


class Sched:
    SEM_LIMIT = 30000
    N_DMA_SEMS = 12

    def __init__(self, nc, stack):
        self.nc = nc
        self.stack = stack
        self.sems = {}
        self.nsem = 0
        self.E = {}
        for name in ("tensor", "vector", "scalar", "gpsimd", "sync"):
            es = EngState(name, getattr(nc, name))
            self.E[name] = es
            self._new_eng_sem(es)
        self.dma_pool = {}
        for q in ("sync", "gpsimd", "scalar"):
            pool = []
            for i in range(self.N_DMA_SEMS):
                pool.append([self._alloc_sem("d_%s_%d" % (q, i)), 0])
            self.dma_pool[q] = [pool, 0]
        self.ninst = 0

    def _alloc_sem(self, name):
        h = self.stack.enter_context(self.nc.semaphore(name))
        key = self.nsem
        self.nsem += 1
        self.sems[key] = h
        return key

    def _new_eng_sem(self, es):
        es.sem = self._alloc_sem("e_%s_%d" % (es.name, self.nsem))
        es.count = 0

    def _wait(self, es, key, val):
        if val <= 0 or es.known.get(key, 0) >= val:
            return
        es.eng.wait_ge(self.sems[key], val)
        es.known[key] = val

    def _deps(self, es, reads, writes):
        toks = {}
        for b in reads:
            if b.w is not None:
                k, v = b.w
                if toks.get(k, 0) < v:
                    toks[k] = v
            if b.x:
                for k, v in b.r.items():
                    if k != es.sem and toks.get(k, 0) < v:
                        toks[k] = v
        for b in writes:
            if b.w is not None:
                k, v = b.w
                if toks.get(k, 0) < v:
                    toks[k] = v
            for k, v in b.r.items():
                if toks.get(k, 0) < v:
                    toks[k] = v
        for k, v in toks.items():
            if k == es.sem and es.name == "tensor":
                continue
            self._wait(es, k, v)

    def _commit(self, tok, reads, writes):
        k, v = tok
        for b in reads:
            if b.r.get(k, 0) < v:
                b.r[k] = v
        for b in writes:
            b.w = tok
            b.r = {}

    def op(self, eng, fn, reads=(), writes=()):
        es = self.E[eng]
        if es.count >= self.SEM_LIMIT:
            self._new_eng_sem(es)
        self._deps(es, reads, writes)
        inst = fn(es.eng)
        es.count += 1
        inst.then_inc(self.sems[es.sem], 1)
        self._commit((es.sem, es.count), reads, writes)
        self.ninst += 1
        return inst

    def dma(self, q, fn, reads=(), writes=()):
        es = self.E[q]
        self._deps(es, reads, writes)
        pool, idx = self.dma_pool[q]
        ent = pool[idx]
        self.dma_pool[q][1] = (idx + 1) % len(pool)
        self._wait(es, ent[0], ent[1])
        inst = fn(es.eng)
        ent[1] += 16
        inst.then_inc(self.sems[ent[0]], 16)
        self._commit((ent[0], ent[1]), reads, writes)
        self.ninst += 1
        return inst

    def barrier(self):
        targets = []
        for es in self.E.values():
            if es.count > 0:
                targets.append((es.sem, es.count))
        for q, (pool, _) in self.dma_pool.items():
            for key, val in pool:
                if val > 0:
                    targets.append((key, val))
        for es in self.E.values():
            for k, v in targets:
                if k == es.sem:
                    continue
                self._wait(es, k, v)


def build(S, CB=3, debug=False, stop=6):
    NB = S // 128
    NOB = NB // 4
    NCH = NOB // 4
    NTOK = NOB * 128
    NBC = NB // 4
    CAP = CB * 128
    NSLOT = NE * CAP
    ZROW = NTOK
    SCALE = 1.0 / math.sqrt(128.0)

    nc = bass.Bass("TRN2", target_bir_lowering=False)

    def din(name, shape, dt=F32):
        return nc.dram_tensor(name, list(shape), dt, kind="ExternalInput").ap()

    def dscr(name, shape, dt):
        return nc.dram_tensor(name, list(shape), dt, kind="ExternalOutput" if debug else "Internal").ap()

    xb = din("xb", [S, D])
    xown = din("xown", [NTOK, D])
    memb = din("memb", [NMEM, D])
    maskd = din("maskd", [4, 128, 128])
    g_mix = din("g_mix", [D]); w_in = din("w_in", [D, 9216]); g_gm = din("g_gm", [1024])
    w_sp = din("w_spatial", [8, 128, 128]); b_sp = din("b_spatial", [8, 128])
    w_bsb = din("w_branch_sb", [1024, D]); w_bgm = din("w_branch_gm", [1024, D]); w_out = din("w_out", [D, D])
    g_cross = din("g_cross", [D]); g_mem = din("g_mem", [D])
    w_xq = din("w_xq", [D, 512]); w_xkv = din("w_xkv", [D, 1024]); w_xo = din("w_xo", [512, D])
    g_ffn = din("g_ffn", [D]); w_rg = din("w_rg", [D, 4]); b_rg = din("b_rg", [4])
    w_re = din("w_re", [D, 32]); b_re = din("b_re", [32])
    if stop >= 5:
        w_eg = din("w_e_gate", [NE, D, DFF]); w_eu = din("w_e_up", [NE, D, DFF]); w_ed = din("w_e_down", [NE, DFF, D])
    g_final = din("g_final", [D])
    yout = nc.dram_tensor("yout", [NTOK, D], F32, kind="ExternalOutput").ap()

    kT_d = dscr("kT_d", [8, 128, S], BF16)
    v_d = dscr("v_d", [S, 1024], BF16)
    qT_d = dscr("qT_d", [8, 128, NTOK], BF16)
    oT_d = dscr("oT_d", [8, 128, NTOK], BF16)
    x2_d = dscr("x2_d", [NTOK, D], F32)
    hf_d = dscr("hf_d", [NTOK + 128, D], BF16)
    rec_d = dscr("rec_d", [NSLOT, 2], I32)
    ys_d = dscr("ys_d", [NSLOT, D], F32)
    b_kT = [[Buf() for _ in range(NBC)] for _ in range(8)]
    b_v = [Buf() for _ in range(NB)]
    b_qT = [[Buf() for _ in range(NCH)] for _ in range(8)]
    b_oT = [[Buf() for _ in range(NCH)] for _ in range(8)]
    b_x2 = [Buf() for _ in range(NOB)]
    b_hf = [Buf() for _ in range(NOB + 1)]
    b_rec = Buf()
    b_ys = [Buf() for _ in range(NE)]

    with ExitStack() as top:
        Sx = Sched(nc, top)
        op = Sx.op
        dma = Sx.dma

        def sbt(st, name, shape, dt):
            return TT(st.enter_context(nc.sbuf_tensor(name, list(shape), dt)))

        PSB = [TT(top.enter_context(nc.psum_tensor("psb%d" % i, [128, 512], F32))) for i in range(6)]
        for p_ in PSB:
            p_.b.x = True
        pst2 = [top.enter_context(nc.psum_tensor("pst%d" % i, [128, 8, 128], BF16)) for i in range(2)]

        class _PST:
            def __getitem__(self, key):
                p, idx, rest = key
                if isinstance(idx, slice):
                    hi = idx.start // 4
                    return pst2[hi][p, idx.start - hi * 4:idx.stop - hi * 4, rest]
                hi = idx // 4
                return pst2[hi][p, idx - hi * 4, rest]

        pst = _PST()
        PT_b = [Buf(True), Buf(True)]
        ps_rr = [0]

        def psn():
            p = PSB[ps_rr[0] % 6]
            ps_rr[0] += 1
            return p

        pt_rr = [0]

        def ptn():
            i = pt_rr[0] % 2
            pt_rr[0] += 1
            return i

        ev_rr = [0]

        def ev_eng():
            ev_rr[0] += 1
            return "vector" if ev_rr[0] % 2 else "scalar"

        def copy_on(eng, out, in_, reads, writes):
            if eng == "scalar":
                op("scalar", lambda e: e.activation(out=out, in_=in_, func=AF.Copy), reads, writes)
            else:
                op(eng, lambda e: e.tensor_copy(out=out, in_=in_), reads, writes)

        def mm(out, lhsT, rhs, start, stop, reads, writes, skip=False):
            op("tensor", lambda e: e.matmul(out, lhsT=lhsT, rhs=rhs, start=start, stop=stop, skip_group_check=skip), reads, writes)

        reg_nslot = nc.gpsimd.to_reg(NSLOT - 1)
        reg_ntok = nc.gpsimd.to_reg(NTOK + 127)
        cst = top
        identf = sbt(cst, "identf", [128, 128], F32)
        ident = sbt(cst, "ident", [128, 128], BF16)
        ones_bf = sbt(cst, "ones_bf", [128, 128], BF16)
        tri_ge = sbt(cst, "tri_ge", [128, 128], BF16)
        ustrict = sbt(cst, "ustrict", [128, 128], BF16)
        trilf = sbt(cst, "trilf", [128, 128], F32)
        gT = {}
        for nm, ap_ in (("mix", g_mix), ("cross", g_cross), ("mem", g_mem), ("ffn", g_ffn)):
            gT[nm] = sbt(cst, "gT_" + nm, [128, 16], F32)
            dma("sync", lambda e: e.dma_start(out=gT[nm].t[:], in_=ap_.rearrange("(c p) -> p c", p=128),
                                              allow_slow_non_contiguous=True), writes=[gT[nm].b])
        mk = sbt(cst, "mk", [128, 4, 128], F32)
        dma("sync", lambda e: e.dma_start(out=mk.t[:], in_=maskd.rearrange("k s t -> s k t")), writes=[mk.b])

        def tri_const(tt_f, cmp, cm, pat):
            op("gpsimd", lambda e: e.memset(tt_f.t[:], 1.0), writes=[tt_f.b])
            op("gpsimd", lambda e: e.affine_select(out=tt_f.t[:], in_=tt_f.t[:], pattern=[[pat, 128]],
                                                   compare_op=cmp, fill=0.0, base=0, channel_multiplier=cm),
               reads=[tt_f.b], writes=[tt_f.b])

        tri_const(identf, ALU.is_equal, 1, -1)
        op("vector", lambda e: e.tensor_copy(out=ident.t[:], in_=identf.t[:]), [identf.b], [ident.b])
        op("vector", lambda e: e.memset(ones_bf.t[:], 1.0), writes=[ones_bf.b])
        tri_const(trilf, ALU.is_ge, 1, -1)
        op("vector", lambda e: e.tensor_copy(out=tri_ge.t[:], in_=trilf.t[:]), [trilf.b], [tri_ge.b])
        tmpf = sbt(cst, "tmpf", [128, 128], F32)
        tri_const(tmpf, ALU.is_gt, -1, 1)
        op("vector", lambda e: e.tensor_copy(out=ustrict.t[:], in_=tmpf.t[:]), [tmpf.b], [ustrict.b])

        def rms_rstd(st_sc, x_ap, xbuf, width, ss, junk):
            op("scalar", lambda e: e.activation(out=junk.t[:, 0:width], in_=x_ap, func=AF.Square, accum_out=ss.t[:, 0:1]),
               [xbuf], [junk.b, ss.b])
            op("vector", lambda e: e.tensor_scalar(out=ss.t[:, 0:1], in0=ss.t[:, 0:1], scalar1=1.0 / width, scalar2=EPS,
                                                   op0=ALU.mult, op1=ALU.add), [ss.b], [ss.b])
            op("scalar", lambda e: e.activation(out=ss.t[:, 0:1], in_=ss.t[:, 0:1], func=AF.Ln), [ss.b], [ss.b])
            op("scalar", lambda e: e.activation(out=ss.t[:, 0:1], in_=ss.t[:, 0:1], func=AF.Exp, scale=-0.5), [ss.b], [ss.b])

        def norm_T(x_ap, xbuf, g_t, hT, col0, ss, junk, xn, ncols=128):
            rms_rstd(None, x_ap, xbuf, D, ss, junk)
            op("vector", lambda e: e.tensor_scalar(out=xn.t[:], in0=x_ap, scalar1=ss.t[:, 0:1], scalar2=None, op0=ALU.mult),
               [xbuf, ss.b], [xn.b])
            for cg in range(4):
                hi = ptn()
                for j in range(4):
                    c = cg * 4 + j
                    op("tensor", lambda e: e.transpose(out=pst[:, hi * 4 + j, 0:ncols], in_=xn.t[0:ncols, c * 128:(c + 1) * 128],
                                                       identity=ident.t[0:ncols, 0:ncols]),
                       [xn.b, ident.b], [PT_b[hi]])
                for j in range(4):
                    c = cg * 4 + j
                    eng = ev_eng()
                    if eng == "vector":
                        op("vector", lambda e: e.tensor_scalar(out=hT.t[:, c, col0:col0 + ncols], in0=pst[:, hi * 4 + j, 0:ncols],
                                                               scalar1=g_t.t[:, c:c + 1], scalar2=None, op0=ALU.mult),
                           [PT_b[hi], g_t.b], [hT.b])
                    else:
                        op("scalar", lambda e: e.activation(out=hT.t[:, c, col0:col0 + ncols], in_=pst[:, hi * 4 + j, 0:ncols],
                                                            func=AF.Copy, scale=g_t.t[:, c:c + 1]),
                           [PT_b[hi], g_t.b], [hT.b])

        NRING = 16
        ring = [sbt(top, "wr%d" % i, [128, 512], BF16) for i in range(NRING)]
        ring_i = [0]

        def wload(src_ap, ncols):
            t = ring[ring_i[0] % NRING]
            ring_i[0] += 1
            dma("gpsimd", lambda e: e.dma_start(out=t.t[:, 0:ncols], in_=src_ap), writes=[t.b])
            return t

        def linear_fm(xT, KC, N, w_ap, col0, ncols, evac):
            for c0 in range(0, ncols, 512):
                nog = min(4, (ncols - c0) // 128)
                banks = [psn() for _ in range(nog)]
                for c in range(KC):
                    wt = wload(w_ap[c * 128:(c + 1) * 128, col0 + c0:col0 + c0 + nog * 128], nog * 128)
                    for j in range(nog):
                        mm(banks[j].t[:, 0:N], wt.t[:, j * 128:(j + 1) * 128], xT.t[:, c, 0:N], c == 0, c == KC - 1,
                           [wt.b, xT.b], [banks[j].b])
                for j in range(nog):
                    evac(c0 // 128 + j, banks[j])

        def linear_tm(xT, KC, nblk, w_ap, col0, ncols, evac):
            for cg in range(ncols // 512):
                banks = [psn() for _ in range(nblk)]
                for c in range(KC):
                    wt = wload(w_ap[c * 128:(c + 1) * 128, col0 + cg * 512:col0 + cg * 512 + 512], 512)
                    for bl in range(nblk):
                        mm(banks[bl].t[:, :], xT.t[:, c, bl * 128:(bl + 1) * 128], wt.t[:, :], c == 0, c == KC - 1,
                           [wt.b, xT.b], [banks[bl].b])
                for bl in range(nblk):
                    evac(bl, cg, banks[bl])

        with ExitStack() as pa:
          if stop >= 1:
            wk = sbt(pa, "wk", [128, 16, 1024], BF16)
            wv = sbt(pa, "wv", [128, 16, 1024], BF16)
            wq = sbt(pa, "wq", [128, 16, 1024], BF16)
            for c in range(16):
                for tt_, cb in ((wq, 0), (wk, 1024), (wv, 2048)):
                    dma("gpsimd", lambda e: e.dma_start(out=tt_.t[:, c, :], in_=w_in[c * 128:(c + 1) * 128, cb:cb + 1024]),
                        writes=[tt_.b])
            xt = [sbt(pa, "xt%d" % i, [128, D], F32) for i in range(2)]
            junk = sbt(pa, "junkA", [128, D], BF16)
            xn = [sbt(pa, "xnA%d" % i, [128, D], BF16) for i in range(2)]
            ss = [sbt(pa, "ssA%d" % i, [128, 1], F32) for i in range(2)]
            hT = [sbt(pa, "hTA%d" % i, [128, 16, 512], BF16) for i in range(2)]
            ksb = [sbt(pa, "ksb%d" % i, [128, 512], BF16) for i in range(3)]
            vsb = [sbt(pa, "vsb%d" % i, [128, 1024], BF16) for i in range(2)]
            xi = 0
            ki = 0
            for ch in range(NBC):
                h_ = hT[ch % 2]
                for bl in range(4):
                    x_ = xt[xi % 2]; xn_ = xn[xi % 2]; ss_ = ss[xi % 2]; xi += 1
                    row = (ch * 4 + bl) * 128
                    dma("sync", lambda e: e.dma_start(out=x_.t[:], in_=xb[row:row + 128, :]), writes=[x_.b])
                    norm_T(x_.t[:], x_.b, gT["mix"], h_, bl * 128, ss_, junk, xn_)
                for h in range(8):
                    pk = psn()
                    for c in range(16):
                        mm(pk.t[:, :], wk.t[:, c, h * 128:(h + 1) * 128], h_.t[:, c, :], c == 0, c == 15, [wk.b, h_.b], [pk.b])
                    k_ = ksb[ki % 3]; ki += 1
                    copy_on(ev_eng(), k_.t[:], pk.t[:, :], [pk.b], [k_.b])
                    dma("sync", lambda e: e.dma_start(out=kT_d[h, :, ch * 512:(ch + 1) * 512], in_=k_.t[:]),
                        reads=[k_.b], writes=[b_kT[h][ch]])
                for bl in range(4):
                    v_ = vsb[bl % 2]
                    for cg in range(2):
                        pv = psn()
                        for c in range(16):
                            mm(pv.t[:, :], h_.t[:, c, bl * 128:(bl + 1) * 128], wv.t[:, c, cg * 512:(cg + 1) * 512], c == 0, c == 15,
                               [wv.b, h_.b], [pv.b])
                        copy_on(ev_eng(), v_.t[:, cg * 512:(cg + 1) * 512], pv.t[:, :], [pv.b], [v_.b])
                    row = (ch * 4 + bl) * 128
                    dma("sync", lambda e: e.dma_start(out=v_d[row:row + 128, :], in_=v_.t[:]), reads=[v_.b], writes=[b_v[ch * 4 + bl]])
            for m in range(NCH):
                h_ = hT[m % 2]
                for bl in range(4):
                    x_ = xt[xi % 2]; xn_ = xn[xi % 2]; ss_ = ss[xi % 2]; xi += 1
                    row = (m * 4 + bl) * 128
                    dma("sync", lambda e: e.dma_start(out=x_.t[:], in_=xown[row:row + 128, :]), writes=[x_.b])
                    norm_T(x_.t[:], x_.b, gT["mix"], h_, bl * 128, ss_, junk, xn_)
                for h in range(8):
                    pk = psn()
                    for c in range(16):
                        mm(pk.t[:, :], wq.t[:, c, h * 128:(h + 1) * 128], h_.t[:, c, :], c == 0, c == 15, [wq.b, h_.b], [pk.b])
                    k_ = ksb[ki % 3]; ki += 1
                    copy_on(ev_eng(), k_.t[:], pk.t[:, :], [pk.b], [k_.b])
                    dma("sync", lambda e: e.dma_start(out=qT_d[h, :, m * 512:(m + 1) * 512], in_=k_.t[:]),
                        reads=[k_.b], writes=[b_qT[h][m]])
            Sx.barrier()

        kxT = sbt(top, "kxT", [128, 4, NMEM], BF16)
        vx = sbt(top, "vx", [128, 2, 512], BF16)
        with ExitStack() as pm:
          if stop >= 2:
            xt = [sbt(pm, "xtM%d" % i, [128, D], F32) for i in range(2)]
            junk = sbt(pm, "junkM", [128, D], BF16)
            xn = [sbt(pm, "xnM%d" % i, [128, D], BF16) for i in range(2)]
            ss = [sbt(pm, "ssM%d" % i, [128, 1], F32) for i in range(2)]
            mT = sbt(pm, "mT", [128, 16, NMEM], BF16)
            for bl in range(2):
                dma("sync", lambda e: e.dma_start(out=xt[bl].t[:], in_=memb[bl * 128:(bl + 1) * 128, :]), writes=[xt[bl].b])
                norm_T(xt[bl].t[:], xt[bl].b, gT["mem"], mT, bl * 128, ss[bl], junk, xn[bl])

            def ev_kx(og, p):
                copy_on(ev_eng(), kxT.t[:, og, :], p.t[:, 0:NMEM], [p.b], [kxT.b])

            linear_fm(mT, 16, NMEM, w_xkv, 0, 512, ev_kx)

            def ev_vx(bl, cg, p):
                copy_on(ev_eng(), vx.t[:, bl, :], p.t[:, :], [p.b], [vx.b])

            linear_tm(mT, 16, 2, w_xkv, 512, 512, ev_vx)
            Sx.barrier()

        with ExitStack() as pb:
          if stop >= 3:
            kTh = [sbt(pb, "kTh%d" % i, [128, S], BF16) for i in range(2)]
            vh = [sbt(pb, "vh%d" % i, [128, NB, 128], BF16) for i in range(2)]
            qTh = [sbt(pb, "qTh%d" % i, [128, NTOK], BF16) for i in range(2)]
            NBUF = 4
            Et = [sbt(pb, "Et%d" % i, [128, 512], BF16) for i in range(NBUF)]
            spt = [sbt(pb, "spt%d" % i, [128, 512], BF16) for i in range(NBUF)]
            Gt = [sbt(pb, "Gt%d" % i, [128, 512], BF16) for i in range(NBUF)]
            At = [sbt(pb, "At%d" % i, [128, 512], BF16) for i in range(NBUF)]
            car = [sbt(pb, "car%d" % i, [128, 512], BF16) for i in range(NBUF)]
            mkb = sbt(pb, "mkb", [128, 4, 128], BF16)
            op("vector", lambda e: e.tensor_copy(out=mkb.t[:], in_=mk.t[:]), [mk.b], [mkb.b])
            sel0 = sbt(pb, "sel0", [128, 128], BF16)
            op("vector", lambda e: e.memset(sel0.t[:], 0.0), writes=[sel0.b])
            op("vector", lambda e: e.memset(sel0.t[0:1, :], 1.0), reads=[sel0.b], writes=[sel0.b])
            osb = [sbt(pb, "osb%d" % i, [128, 512], BF16) for i in range(2)]
            v_view = v_d.rearrange("(n p) (h d) -> h p n d", p=128, d=128)

            def load_head(h):
                i = h % 2
                dma("sync", lambda e: e.dma_start(out=kTh[i].t[:], in_=kT_d[h]), reads=[b for b in b_kT[h]], writes=[kTh[i].b])
                for n0 in range(0, NB, 8):
                    dma("sync", lambda e: e.dma_start(out=vh[i].t[:, n0:n0 + 8, :], in_=v_view[h][:, n0:n0 + 8, :]), reads=b_v, writes=[vh[i].b])
                dma("sync", lambda e: e.dma_start(out=qTh[i].t[:], in_=qT_d[h]), reads=[b for b in b_qT[h]], writes=[qTh[i].b])

            load_head(0)
            itc = [0]
            oi = 0
            for h in range(8):
                if h + 1 < 8:
                    load_head(h + 1)
                hi = h % 2
                for m in range(NCH):
                    po = PSB[oi % 2]
                    its = []
                    for kb in range(16 * m + 15, -1, -1):
                        if kb >= 16 * m:
                            its.append((kb, 128 * ((kb - 16 * m) // 4), (kb - 16 * m) % 4, True))
                        else:
                            its.append((kb, 0, 0, False))
                    base = itc[0]
                    itc[0] += len(its)

                    def front(n):
                        kb, c0, k, diag = its[n]
                        i = (base + n) % NBUF
                        pS = PSB[2 + (base + n) % 2]
                        mm(pS.t[:, c0:512], kTh[hi].t[:, kb * 128:(kb + 1) * 128], qTh[hi].t[:, m * 512 + c0:(m + 1) * 512],
                           True, True, [kTh[hi].b, qTh[hi].b], [pS.b])
                        op("scalar", lambda e: e.activation(out=Et[i].t[:, c0:512], in_=pS.t[:, c0:512], func=AF.Exp, scale=SCALE),
                           [pS.b], [Et[i].b])
                        if diag:
                            op("vector", lambda e: e.tensor_tensor(out=Et[i].t[:, c0:c0 + 128], in0=Et[i].t[:, c0:c0 + 128],
                                                                  in1=mkb.t[:, k, :], op=ALU.mult), [Et[i].b, mkb.b], [Et[i].b])
                        op("scalar", lambda e: e.activation(out=spt[i].t[:, c0:512], in_=Et[i].t[:, c0:512], func=AF.Ln, bias=1.0),
                           [Et[i].b], [spt[i].b])

                    def back(n):
                        kb, c0, k, diag = its[n]
                        i = (base + n) % NBUF
                        pC = PSB[4 + (base + n) % 2]
                        has_carry = n > 0
                        mm(pC.t[:, c0:512], tri_ge.t[:, :], spt[i].t[:, c0:512], True, not has_carry, [tri_ge.b, spt[i].b], [pC.b], skip=True)
                        if has_carry:
                            pc0 = its[n - 1][1]
                            ip = (base + n - 1) % NBUF
                            mm(pC.t[:, pc0:512], sel0.t[:, :], car[ip].t[:, pc0:512], False, True, [sel0.b, car[ip].b], [pC.b], skip=True)
                        if n + 1 < len(its):
                            op("vector", lambda e: e.tensor_copy(out=car[i].t[:, c0:512], in_=pC.t[:, c0:512]), [pC.b], [car[i].b])

                    def back2(n):
                        kb, c0, k, diag = its[n]
                        i = (base + n) % NBUF
                        pC = PSB[4 + (base + n) % 2]
                        op("scalar", lambda e: e.activation(out=Gt[i].t[:, c0:512], in_=pC.t[:, c0:512], func=AF.Exp, scale=-1.0),
                           [pC.b], [Gt[i].b])
                        op("vector", lambda e: e.tensor_tensor(out=At[i].t[:, c0:512], in0=Et[i].t[:, c0:512], in1=Gt[i].t[:, c0:512],
                                                              op=ALU.mult), [Et[i].b, Gt[i].b], [At[i].b])
                        mm(po.t[:, c0:512], vh[hi].t[:, kb, :], At[i].t[:, c0:512], n == 0, n == len(its) - 1, [vh[hi].b, At[i].b], [po.b], skip=True)

                    front(0)
                    back(0)
                    for n in range(len(its)):
                        if n + 1 < len(its):
                            front(n + 1)
                            back(n + 1)
                        back2(n)
                    o_ = osb[oi % 2]; oi += 1
                    op("vector", lambda e: e.tensor_copy(out=o_.t[:], in_=po.t[:, :]), [po.b], [o_.b])
                    dma("sync", lambda e: e.dma_start(out=oT_d[h, :, m * 512:(m + 1) * 512], in_=o_.t[:]),
                        reads=[o_.b], writes=[b_oT[h][m]])
            Sx.barrier()

        slots = sbt(top, "slots", [128, NOB, 2], I32)
        with ExitStack() as pc:
          if stop >= 4:
            xres = sbt(pc, "xres", [128, 4, D], F32)
            hT = sbt(pc, "hTC", [128, 16, 512], BF16)
            junk = sbt(pc, "junkC", [128, D], BF16)
            xn = [sbt(pc, "xnC%d" % i, [128, D], BF16) for i in range(2)]
            ss = [sbt(pc, "ssC%d" % i, [128, 1], F32) for i in range(2)]
            uT = sbt(pc, "uT", [128, 8, 512], BF16)
            vtm = [sbt(pc, "vtm%d" % i, [128, 1024], F32) for i in range(2)]
            vnb = [sbt(pc, "vnb%d" % i, [128, 1024], BF16) for i in range(2)]
            ogT = sbt(pc, "ogT", [128, 8, 512], BF16)
            oT = sbt(pc, "oT", [128, 8, 512], BF16)
            mA = sbt(pc, "mA", [128, 16, 512], BF16)
            mB = sbt(pc, "mB", [128, 16, 512], BF16)
            sg = [sbt(pc, "sg%d" % i, [128, 512], F32) for i in range(2)]
            ggm_bc = sbt(pc, "ggm_bc", [128, 1024], F32)
            bias_rep = sbt(pc, "bias_rep", [128, 8, 128], F32)
            wmT = sbt(pc, "wmT", [128, 8, 128], BF16)
            qxT = sbt(pc, "qxT", [128, 4, 512], BF16)
            oxT = sbt(pc, "oxT", [128, 4, 512], BF16)
            Pf = sbt(pc, "Pf", [128, 4, NMEM], F32)
            Pn = sbt(pc, "Pn", [128, 4, NMEM], BF16)
            PTs = sbt(pc, "PTs", [128, 8, 128], BF16)
            sm = sbt(pc, "sm", [128, 16], F32)
            wr = sbt(pc, "wr", [128, 16, 36], BF16)
            wrf = sbt(pc, "wrf", [128, 16, 36], F32)
            rb = sbt(pc, "rb", [128, 36], F32)
            eoff = sbt(pc, "eoff", [128, 32], F32)
            tokf = sbt(pc, "tokf", [128, 1], F32)
            cum = sbt(pc, "cum", [128, 32], F32)
            cum_bf = sbt(pc, "cum_bf", [128, 32], BF16)
            R = sbt(pc, "R", [128, 200], F32)
            msk_bf = sbt(pc, "msk_bf", [128, 32], BF16)
            rec = [sbt(pc, "rec%d" % i, [128, 2], I32) for i in range(4)]
            sl_i = [sbt(pc, "sl_i%d" % i, [128, 1], I32) for i in range(4)]
            wsp_f = TT(vtm[0].t.rearrange("p (g s) -> p g s", g=8)); wsp_f.b = vtm[0].b
            wsp_b = TT(vnb[0].t.rearrange("p (g s) -> p g s", g=8)); wsp_b.b = vnb[0].b
            zrow = junk
            reci = sbt(pc, "reci", [128, NSLOT // 128, 2], I32)

            dma("sync", lambda e: e.dma_start(out=ggm_bc.t[:], in_=g_gm.partition_broadcast(128)), writes=[ggm_bc.b])
            dma("sync", lambda e: e.dma_start(out=bias_rep.t[:], in_=b_sp.partition_broadcast(128)), writes=[bias_rep.b])
            dma("sync", lambda e: e.dma_start(out=wsp_f.t[:], in_=w_sp.rearrange("g t s -> t g s")), writes=[wsp_f.b])
            for g in range(8):
                op("gpsimd", lambda e: e.tensor_tensor(out=wsp_f.t[:, g, :], in0=wsp_f.t[:, g, :], in1=trilf.t[:, :], op=ALU.mult),
                   [wsp_f.b, trilf.b], [wsp_f.b])
            op("vector", lambda e: e.tensor_copy(out=wsp_b.t[:], in_=wsp_f.t[:]), [wsp_f.b], [wsp_b.b])
            for g in range(8):
                hi = ptn()
                op("tensor", lambda e: e.transpose(out=pst[:, hi * 4, :], in_=wsp_b.t[:, g, :], identity=ident.t[:, :]),
                   [wsp_b.b, ident.b], [PT_b[hi]])
                op("vector", lambda e: e.tensor_copy(out=wmT.t[:, g, :], in_=pst[:, hi * 4, :]), [PT_b[hi]], [wmT.b])
            dma("sync", lambda e: e.dma_start(out=wrf.t[:, :, 0:4], in_=w_rg.rearrange("(c p) n -> p c n", p=128)), writes=[wrf.b])
            dma("sync", lambda e: e.dma_start(out=wrf.t[:, :, 4:36], in_=w_re.rearrange("(c p) n -> p c n", p=128)), writes=[wrf.b])
            op("vector", lambda e: e.tensor_copy(out=wr.t[:], in_=wrf.t[:]), [wrf.b], [wr.b])
            dma("sync", lambda e: e.dma_start(out=rb.t[:, 0:4], in_=b_rg.partition_broadcast(128)), writes=[rb.b])
            dma("sync", lambda e: e.dma_start(out=rb.t[:, 4:36], in_=b_re.partition_broadcast(128)), writes=[rb.b])
            op("gpsimd", lambda e: e.iota(eoff.t[:], pattern=[[CAP, 32]], base=0, channel_multiplier=0,
                                          allow_small_or_imprecise_dtypes=True), writes=[eoff.b])
            op("gpsimd", lambda e: e.iota(tokf.t[:], pattern=[[0, 1]], base=0, channel_multiplier=1,
                                          allow_small_or_imprecise_dtypes=True), writes=[tokf.b])
            op("vector", lambda e: e.memset(cum.t[:], 0.0), writes=[cum.b])
            op("vector", lambda e: e.memset(zrow.t[:], 0.0), writes=[zrow.b])
            dma("sync", lambda e: e.dma_start(out=hf_d[NTOK:NTOK + 128, :], in_=zrow.t[:]), reads=[zrow.b], writes=[b_hf[NOB]])
            op("vector", lambda e: e.memset(reci.t[:, :, 0:1], ZROW), writes=[reci.b])
            op("vector", lambda e: e.memset(reci.t[:, :, 1:2], 0), writes=[reci.b])
            dma("sync", lambda e: e.dma_start(out=rec_d.rearrange("(p n) c -> p n c", p=128), in_=reci.t[:]),
                reads=[reci.b], writes=[b_rec])

            ri = 0
            for m in range(NCH):
                for bl in range(4):
                    row = (m * 4 + bl) * 128
                    dma("sync", lambda e: e.dma_start(out=xres.t[:, bl, :], in_=xown[row:row + 128, :]), writes=[xres.b])
                dma("sync", lambda e: e.dma_start(out=oT.t[:], in_=oT_d[:, :, m * 512:(m + 1) * 512].rearrange("h p n -> p h n")),
                    reads=[b_oT[h][m] for h in range(8)], writes=[oT.b])
                for bl in range(4):
                    norm_T(xres.t[:, bl, :], xres.b, gT["mix"], hT, bl * 128, ss[bl % 2], junk, xn[bl % 2])

                def ev_u(og, p):
                    op("scalar", lambda e: e.activation(out=uT.t[:, og, :], in_=p.t[:, :], func=AF.Gelu), [p.b], [uT.b])

                linear_fm(hT, 16, 512, w_in, 3072, 1024, ev_u)

                def ev_v(bl, cg, p):
                    v_ = vtm[bl % 2]
                    op("scalar", lambda e: e.activation(out=v_.t[:, cg * 512:(cg + 1) * 512], in_=p.t[:, :], func=AF.Gelu),
                       [p.b], [v_.b])
                    if cg == 1:
                        s_ = ss[bl % 2]
                        rms_rstd(None, v_.t[:], v_.b, 1024, s_, junk)
                        op("vector", lambda e: e.scalar_tensor_tensor(out=vnb[bl % 2].t[:], in0=v_.t[:], scalar=s_.t[:, 0:1],
                                                                      in1=ggm_bc.t[:], op0=ALU.mult, op1=ALU.mult),
                           [v_.b, s_.b, ggm_bc.b], [vnb[bl % 2].b])

                for bl in range(4):
                    for cg in range(2):
                        pbank = psn()
                        for c in range(16):
                            wt = wload(w_in[c * 128:(c + 1) * 128, 4096 + cg * 512:4096 + cg * 512 + 512], 512)
                            mm(pbank.t[:, :], hT.t[:, c, bl * 128:(bl + 1) * 128], wt.t[:, :], c == 0, c == 15, [wt.b, hT.b], [pbank.b])
                        ev_v(bl, cg, pbank)
                    for gh in range(2):
                        pm_ = psn()
                        for gg in range(4):
                            g = gh * 4 + gg
                            mm(pm_.t[:, gg * 128:(gg + 1) * 128], vnb[bl % 2].t[:, g * 128:(g + 1) * 128], wmT.t[:, g, :], True, True,
                               [vnb[bl % 2].b, wmT.b], [pm_.b])
                        for gg in range(4):
                            g = gh * 4 + gg
                            s_ = sg[(g + bl) % 2]
                            op("vector", lambda e: e.tensor_tensor(out=s_.t[:, 0:128], in0=pm_.t[:, gg * 128:(gg + 1) * 128],
                                                                  in1=bias_rep.t[:, g, 0:128], op=ALU.add), [pm_.b, bias_rep.b], [s_.b])
                            op("gpsimd", lambda e: e.tensor_tensor(out=ogT.t[:, g, bl * 128:(bl + 1) * 128], in0=s_.t[:, 0:128],
                                                                  in1=uT.t[:, g, bl * 128:(bl + 1) * 128], op=ALU.mult),
                               [s_.b, uT.b], [ogT.b])

                def ev_store(dst):
                    def f(og, p):
                        copy_on(ev_eng(), dst.t[:, og, :], p.t[:, :], [p.b], [dst.b])
                    return f

                def ev_gate(dst):
                    def f(og, p):
                        s_ = sg[og % 2]
                        op("scalar", lambda e: e.activation(out=s_.t[:, :], in_=p.t[:, :], func=AF.Sigmoid), [p.b], [s_.b])
                        op("gpsimd", lambda e: e.tensor_tensor(out=dst.t[:, og, :], in0=dst.t[:, og, :], in1=s_.t[:, :], op=ALU.mult),
                           [s_.b, dst.b], [dst.b])
                    return f

                linear_fm(oT, 8, 512, w_bsb, 0, D, ev_store(mA))
                linear_fm(hT, 16, 512, w_in, 5120, D, ev_gate(mA))
                linear_fm(ogT, 8, 512, w_bgm, 0, D, ev_store(mB))
                linear_fm(hT, 16, 512, w_in, 7168, D, ev_gate(mB))
                for og in range(16):
                    op("vector" if og % 2 else "gpsimd",
                       lambda e: e.tensor_tensor(out=mA.t[:, og, :], in0=mA.t[:, og, :], in1=mB.t[:, og, :], op=ALU.add),
                       [mA.b, mB.b], [mA.b])

                def ev_res(bl, cg, p):
                    op("vector", lambda e: e.tensor_tensor(out=xres.t[:, bl, cg * 512:(cg + 1) * 512], in0=p.t[:, :],
                                                          in1=xres.t[:, bl, cg * 512:(cg + 1) * 512], op=ALU.add),
                       [p.b, xres.b], [xres.b])

                linear_tm(mA, 16, 4, w_out, 0, D, ev_res)

                for bl in range(4):
                    norm_T(xres.t[:, bl, :], xres.b, gT["cross"], hT, bl * 128, ss[bl % 2], junk, xn[bl % 2])
                linear_fm(hT, 16, 512, w_xq, 0, 512, ev_store(qxT))
                XS = 1.0 / math.sqrt(128.0)
                for bl in range(4):
                    pS2 = [psn(), psn()]
                    for hh in range(4):
                        p_ = pS2[hh // 2]
                        mm(p_.t[:, (hh % 2) * 256:(hh % 2) * 256 + 256], qxT.t[:, hh, bl * 128:(bl + 1) * 128], kxT.t[:, hh, :],
                           True, True, [qxT.b, kxT.b], [p_.b])
                    for hh in range(4):
                        p_ = pS2[hh // 2]
                        op("vector", lambda e: e.reduce_max(out=sm.t[:, hh:hh + 1], in_=p_.t[:, (hh % 2) * 256:(hh % 2) * 256 + 256],
                                                           axis=AX.X), [p_.b], [sm.b])
                    op("vector", lambda e: e.tensor_scalar(out=sm.t[:, 4:8], in0=sm.t[:, 0:4], scalar1=-XS, scalar2=None, op0=ALU.mult),
                       [sm.b], [sm.b])
                    for hh in range(4):
                        p_ = pS2[hh // 2]
                        op("scalar", lambda e: e.activation(out=Pf.t[:, hh, :], in_=p_.t[:, (hh % 2) * 256:(hh % 2) * 256 + 256],
                                                            func=AF.Exp, bias=sm.t[:, 4 + hh:5 + hh], scale=XS,
                                                            accum_out=sm.t[:, 8 + hh:9 + hh]), [p_.b, sm.b], [Pf.b, sm.b])
                    op("vector", lambda e: e.reciprocal(out=sm.t[:, 12:16], in_=sm.t[:, 8:12]), [sm.b], [sm.b])
                    for hh in range(4):
                        op("vector", lambda e: e.tensor_scalar(out=Pn.t[:, hh, :], in0=Pf.t[:, hh, :], scalar1=sm.t[:, 12 + hh:13 + hh],
                                                               scalar2=None, op0=ALU.mult), [Pf.b, sm.b], [Pn.b])
                    for half in range(2):
                        hi = ptn()
                        for j in range(4):
                            hh = half * 2 + j // 2
                            mc = j % 2
                            op("tensor", lambda e: e.transpose(out=pst[:, hi * 4 + j, :], in_=Pn.t[:, hh, mc * 128:(mc + 1) * 128],
                                                               identity=ident.t[:, :]), [Pn.b, ident.b], [PT_b[hi]])
                        copy_on(ev_eng(), PTs.t[:, half * 4:half * 4 + 4, :], pst[:, hi * 4:hi * 4 + 4, :], [PT_b[hi]], [PTs.b])
                    pO = psn()
                    for hh in range(4):
                        for mc in range(2):
                            mm(pO.t[:, hh * 128:(hh + 1) * 128], vx.t[:, mc, hh * 128:(hh + 1) * 128], PTs.t[:, hh * 2 + mc, :],
                               mc == 0, mc == 1, [vx.b, PTs.b], [pO.b])
                    copy_on(ev_eng(), oxT.t[:, :, bl * 128:(bl + 1) * 128], pO.t[:, :].rearrange("p (h n) -> p h n", h=4),
                            [pO.b], [oxT.b])
                linear_tm(oxT, 4, 4, w_xo, 0, D, ev_res)

                for bl in range(4):
                    ob = m * 4 + bl
                    row = ob * 128
                    xn_ = xn[bl % 2]
                    norm_T(xres.t[:, bl, :], xres.b, gT["ffn"], hT, bl * 128, ss[bl % 2], junk, xn_)
                    dma("sync", lambda e: e.dma_start(out=x2_d[row:row + 128, :], in_=xres.t[:, bl, :]), reads=[xres.b], writes=[b_x2[ob]])
                    dma("sync", lambda e: e.dma_start(out=hf_d[row:row + 128, :], in_=xn_.t[:]), reads=[xn_.b], writes=[b_hf[ob]])
                    pl = psn()
                    for c in range(16):
                        mm(pl.t[:, 0:36], hT.t[:, c, bl * 128:(bl + 1) * 128], wr.t[:, c, :], c == 0, c == 15, [hT.b, wr.b], [pl.b])
                    Rt = R.t
                    LG, GM, OHG, EL, M1, OH1, EL2, M2, OH2, P8 = 0, 36, 40, 44, 52, 56, 64, 72, 76, 84
                    NGM, SUMG, PG, DD, W1, W2, OH12, M32, POS, SL1, SL2, TOK = 92, 93, 94, 95, 96, 97, 98, 106, 138, 150, 151, 152
                    rbuf = [R.b]

                    def V(fn, extra_r=(), extra_w=()):
                        op("vector", fn, rbuf + list(extra_r), rbuf + list(extra_w))

                    V(lambda e: e.tensor_tensor(out=Rt[:, LG:LG + 36], in0=pl.t[:, 0:36], in1=rb.t[:, :], op=ALU.add), [pl.b, rb.b])
                    V(lambda e: e.reduce_max(out=Rt[:, GM:GM + 1], in_=Rt[:, LG:LG + 4], axis=AX.X))
                    V(lambda e: e.tensor_scalar(out=Rt[:, OHG:OHG + 4], in0=Rt[:, LG:LG + 4], scalar1=Rt[:, GM:GM + 1], scalar2=None,
                                                op0=ALU.is_equal))
                    V(lambda e: e.tensor_scalar(out=Rt[:, NGM:NGM + 1], in0=Rt[:, GM:GM + 1], scalar1=-1.0, scalar2=None, op0=ALU.mult))
                    op("scalar", lambda e: e.activation(out=Rt[:, P8:P8 + 4], in_=Rt[:, LG:LG + 4], func=AF.Exp, bias=Rt[:, NGM:NGM + 1],
                                                        scale=1.0, accum_out=Rt[:, SUMG:SUMG + 1]), rbuf, rbuf)
                    V(lambda e: e.reciprocal(out=Rt[:, PG:PG + 1], in_=Rt[:, SUMG:SUMG + 1]))
                    V(lambda e: e.tensor_scalar(out=Rt[:, EL:EL + 8], in0=Rt[:, LG + 4:LG + 12], scalar1=Rt[:, OHG:OHG + 1], scalar2=None,
                                                op0=ALU.mult))
                    for g in range(1, 4):
                        V(lambda e: e.scalar_tensor_tensor(out=Rt[:, EL:EL + 8], in0=Rt[:, LG + 4 + 8 * g:LG + 12 + 8 * g],
                                                           scalar=Rt[:, OHG + g:OHG + g + 1], in1=Rt[:, EL:EL + 8],
                                                           op0=ALU.mult, op1=ALU.add))
                    V(lambda e: e.reduce_max(out=Rt[:, M1:M1 + 1], in_=Rt[:, EL:EL + 8], axis=AX.X))
                    V(lambda e: e.tensor_scalar(out=Rt[:, OH1:OH1 + 8], in0=Rt[:, EL:EL + 8], scalar1=Rt[:, M1:M1 + 1], scalar2=None,
                                                op0=ALU.is_equal))
                    V(lambda e: e.scalar_tensor_tensor(out=Rt[:, EL2:EL2 + 8], in0=Rt[:, OH1:OH1 + 8], scalar=-1e30,
                                                       in1=Rt[:, EL:EL + 8], op0=ALU.mult, op1=ALU.add))
                    V(lambda e: e.reduce_max(out=Rt[:, M2:M2 + 1], in_=Rt[:, EL2:EL2 + 8], axis=AX.X))
                    V(lambda e: e.tensor_scalar(out=Rt[:, OH2:OH2 + 8], in0=Rt[:, EL2:EL2 + 8], scalar1=Rt[:, M2:M2 + 1], scalar2=None,
                                                op0=ALU.is_equal))
                    V(lambda e: e.tensor_tensor(out=Rt[:, DD:DD + 1], in0=Rt[:, M2:M2 + 1], in1=Rt[:, M1:M1 + 1], op=ALU.subtract))
                    op("scalar", lambda e: e.activation(out=Rt[:, DD:DD + 1], in_=Rt[:, DD:DD + 1], func=AF.Exp), rbuf, rbuf)
                    V(lambda e: e.tensor_scalar(out=Rt[:, DD:DD + 1], in0=Rt[:, DD:DD + 1], scalar1=1.0, scalar2=None, op0=ALU.add))
                    V(lambda e: e.reciprocal(out=Rt[:, W1:W1 + 1], in_=Rt[:, DD:DD + 1]))
                    V(lambda e: e.tensor_tensor(out=Rt[:, W1:W1 + 1], in0=Rt[:, W1:W1 + 1], in1=Rt[:, PG:PG + 1], op=ALU.mult))
                    V(lambda e: e.tensor_tensor(out=Rt[:, W2:W2 + 1], in0=Rt[:, PG:PG + 1], in1=Rt[:, W1:W1 + 1], op=ALU.subtract))
                    V(lambda e: e.tensor_tensor(out=Rt[:, OH12:OH12 + 8], in0=Rt[:, OH1:OH1 + 8], in1=Rt[:, OH2:OH2 + 8], op=ALU.add))
                    for g in range(4):
                        V(lambda e: e.tensor_scalar(out=Rt[:, M32 + 8 * g:M32 + 8 * g + 8], in0=Rt[:, OH12:OH12 + 8],
                                                    scalar1=Rt[:, OHG + g:OHG + g + 1], scalar2=None, op0=ALU.mult))
                    op("vector", lambda e: e.tensor_copy(out=msk_bf.t[:], in_=Rt[:, M32:M32 + 32]), rbuf, [msk_bf.b])
                    pp = psn()
                    mm(pp.t[:, 0:32], ustrict.t[:, :], msk_bf.t[:, :], True, ob == 0, [ustrict.b, msk_bf.b], [pp.b])
                    if ob > 0:
                        mm(pp.t[:, 0:32], ones_bf.t[:, :], cum_bf.t[:, :], False, True, [ones_bf.b, cum_bf.b], [pp.b])
                    PC = 160
                    V(lambda e: e.tensor_scalar(out=Rt[:, PC:PC + 32], in0=pp.t[:, 0:32], scalar1=float(CAP - 1), scalar2=None, op0=ALU.min),
                      [pp.b])
                    V(lambda e: e.tensor_tensor(out=Rt[:, PC:PC + 32], in0=Rt[:, PC:PC + 32], in1=eoff.t[:, :], op=ALU.add), [eoff.b])
                    op("vector", lambda e: e.tensor_tensor(out=cum.t[:], in0=cum.t[:], in1=Rt[:, M32:M32 + 32], op=ALU.add),
                       rbuf + [cum.b], [cum.b])
                    op("vector", lambda e: e.tensor_copy(out=cum_bf.t[:], in_=cum.t[:]), [cum.b], [cum_bf.b])
                    V(lambda e: e.tensor_scalar(out=Rt[:, P8:P8 + 8], in0=Rt[:, PC:PC + 8], scalar1=Rt[:, OHG:OHG + 1], scalar2=None,
                                                op0=ALU.mult))
                    for g in range(1, 4):
                        V(lambda e: e.scalar_tensor_tensor(out=Rt[:, P8:P8 + 8], in0=Rt[:, PC + 8 * g:PC + 8 * g + 8],
                                                           scalar=Rt[:, OHG + g:OHG + g + 1], in1=Rt[:, P8:P8 + 8],
                                                           op0=ALU.mult, op1=ALU.add))
                    V(lambda e: e.tensor_tensor(out=Rt[:, EL:EL + 8], in0=Rt[:, OH1:OH1 + 8], in1=Rt[:, P8:P8 + 8], op=ALU.mult))
                    V(lambda e: e.reduce_sum(out=Rt[:, SL1:SL1 + 1], in_=Rt[:, EL:EL + 8], axis=AX.X))
                    V(lambda e: e.tensor_tensor(out=Rt[:, EL2:EL2 + 8], in0=Rt[:, OH2:OH2 + 8], in1=Rt[:, P8:P8 + 8], op=ALU.mult))
                    V(lambda e: e.reduce_sum(out=Rt[:, SL2:SL2 + 1], in_=Rt[:, EL2:EL2 + 8], axis=AX.X))
                    V(lambda e: e.tensor_scalar(out=Rt[:, TOK:TOK + 1], in0=tokf.t[:, 0:1], scalar1=float(row), scalar2=None, op0=ALU.add),
                      [tokf.b])
                    op("vector", lambda e: e.tensor_copy(out=slots.t[:, ob, 0:1], in_=Rt[:, SL1:SL1 + 1]), rbuf, [slots.b])
                    op("vector", lambda e: e.tensor_copy(out=slots.t[:, ob, 1:2], in_=Rt[:, SL2:SL2 + 1]), rbuf, [slots.b])
                    for ch_, (slc, wc) in enumerate(((SL1, W1), (SL2, W2))):
                        rc = rec[ri % 4]; si = sl_i[ri % 4]; ri += 1
                        rcf = rc.t.bitcast(F32)
                        op("vector", lambda e: e.tensor_copy(out=rc.t[:, 0:1], in_=Rt[:, TOK:TOK + 1]), rbuf, [rc.b])
                        op("vector", lambda e: e.tensor_copy(out=rcf[:, 1:2], in_=Rt[:, wc:wc + 1]), rbuf, [rc.b])
                        op("vector", lambda e: e.tensor_copy(out=si.t[:, 0:1], in_=Rt[:, slc:slc + 1]), rbuf, [si.b])
                        dma("gpsimd", lambda e: e.indirect_dma_start(out=rec_d[:, :],
                                                                     out_offset=bass.IndirectOffsetOnAxis(ap=si.t[:, 0:1], axis=0),
                                                                     in_=rc.t[:, :], in_offset=None,
                                                                     bounds_check=reg_nslot, oob_is_err=False),
                            reads=[rc.b, si.b], writes=[b_rec])
            Sx.barrier()

        with ExitStack() as pd:
          if stop >= 5:
            idx = [sbt(pd, "idx%d" % i, [128, CB, 2], I32) for i in range(2)]
            xg = [sbt(pd, "xg%d" % i, [128, D], BF16) for i in range(3)]
            xT = [sbt(pd, "xTD%d" % i, [128, 16, CAP], BF16) for i in range(2)]
            aT = [sbt(pd, "aT%d" % i, [128, 8, CAP], BF16) for i in range(2)]
            sl = [sbt(pd, "sl%d" % i, [128, CAP], F32) for i in range(4)]
            ysb = [sbt(pd, "ysb%d" % i, [128, 512], F32) for i in range(4)]
            sli = 0
            yi = 0
            gi = 0
            for ex in range(NE):
                id_ = idx[ex % 2]
                x_T = xT[ex % 2]
                a_T = aT[ex % 2]
                dma("sync", lambda e: e.dma_start(out=id_.t[:], in_=rec_d[ex * CAP:(ex + 1) * CAP, :].rearrange("(b p) c -> p b c", p=128)),
                    reads=[b_rec], writes=[id_.b])
                for bl in range(CB):
                    g_ = xg[gi % 3]; gi += 1
                    dma("gpsimd", lambda e: e.indirect_dma_start(out=g_.t[:, :], out_offset=None, in_=hf_d[:, :],
                                                                 in_offset=bass.IndirectOffsetOnAxis(ap=id_.t[:, bl, 0:1], axis=0),
                                                                 bounds_check=reg_ntok, oob_is_err=False),
                        reads=[id_.b] + b_hf, writes=[g_.b])
                    for cg in range(4):
                        hi = ptn()
                        for j in range(4):
                            c = cg * 4 + j
                            op("tensor", lambda e: e.transpose(out=pst[:, hi * 4 + j, :], in_=g_.t[:, c * 128:(c + 1) * 128],
                                                               identity=ident.t[:, :]), [g_.b, ident.b], [PT_b[hi]])
                        for j in range(4):
                            c = cg * 4 + j
                            eng = ev_eng()
                            if eng == "vector":
                                op("vector", lambda e: e.tensor_scalar(out=x_T.t[:, c, bl * 128:(bl + 1) * 128], in0=pst[:, hi * 4 + j, :],
                                                                       scalar1=gT["ffn"].t[:, c:c + 1], scalar2=None, op0=ALU.mult),
                                   [PT_b[hi], gT["ffn"].b], [x_T.b])
                            else:
                                op("scalar", lambda e: e.activation(out=x_T.t[:, c, bl * 128:(bl + 1) * 128], in_=pst[:, hi * 4 + j, :],
                                                                    func=AF.Copy, scale=gT["ffn"].t[:, c:c + 1]),
                                   [PT_b[hi], gT["ffn"].b], [x_T.b])
                for fg in range(2):
                    silus = []
                    banks1 = [psn() for _ in range(4)]
                    for c in range(16):
                        wt = wload(w_eg[ex, c * 128:(c + 1) * 128, fg * 512:(fg + 1) * 512], 512)
                        for j in range(4):
                            mm(banks1[j].t[:, 0:CAP], wt.t[:, j * 128:(j + 1) * 128], x_T.t[:, c, :], c == 0, c == 15, [wt.b, x_T.b], [banks1[j].b])
                    for j in range(4):
                        s_ = sl[sli % 4]; sli += 1
                        op("scalar", lambda e: e.activation(out=s_.t[:, :], in_=banks1[j].t[:, 0:CAP], func=AF.Silu), [banks1[j].b], [s_.b])
                        silus.append(s_)
                    banks2 = [psn(), psn(), banks1[0], banks1[1]]
                    for c in range(16):
                        wt = wload(w_eu[ex, c * 128:(c + 1) * 128, fg * 512:(fg + 1) * 512], 512)
                        for j in range(4):
                            mm(banks2[j].t[:, 0:CAP], wt.t[:, j * 128:(j + 1) * 128], x_T.t[:, c, :], c == 0, c == 15, [wt.b, x_T.b], [banks2[j].b])
                    for j in range(4):
                        f = fg * 4 + j
                        op("vector", lambda e: e.tensor_tensor(out=a_T.t[:, f, :], in0=banks2[j].t[:, 0:CAP], in1=silus[j].t[:, :], op=ALU.mult),
                           [banks2[j].b, silus[j].b], [a_T.b])
                idf = id_.t.bitcast(F32)
                for cg in range(4):
                    banks = [psn() for _ in range(CB)]
                    for f in range(8):
                        wt = wload(w_ed[ex, f * 128:(f + 1) * 128, cg * 512:(cg + 1) * 512], 512)
                        for bl in range(CB):
                            mm(banks[bl].t[:, :], a_T.t[:, f, bl * 128:(bl + 1) * 128], wt.t[:, :], f == 0, f == 7, [wt.b, a_T.b], [banks[bl].b])
                    for bl in range(CB):
                        y_ = ysb[yi % 4]; yi += 1
                        eng = ev_eng()
                        if eng == "vector":
                            op("vector", lambda e: e.tensor_scalar(out=y_.t[:, 0:512], in0=banks[bl].t[:, :], scalar1=idf[:, bl, 1:2],
                                                                   scalar2=None, op0=ALU.mult), [banks[bl].b, id_.b], [y_.b])
                        else:
                            op("scalar", lambda e: e.activation(out=y_.t[:, 0:512], in_=banks[bl].t[:, :], func=AF.Copy, scale=idf[:, bl, 1:2]),
                               [banks[bl].b, id_.b], [y_.b])
                        r0 = ex * CAP + bl * 128
                        dma("sync", lambda e: e.dma_start(out=ys_d[r0:r0 + 128, cg * 512:(cg + 1) * 512], in_=y_.t[:, 0:512]),
                            reads=[y_.b], writes=[])
            Sx.barrier()

        with ExitStack() as pe:
          if stop >= 6:
            gfin = sbt(pe, "gfin", [128, D], F32)
            dma("sync", lambda e: e.dma_start(out=gfin.t[:], in_=g_final.partition_broadcast(128)), writes=[gfin.b])
            x2 = [sbt(pe, "x2_%d" % i, [128, D], F32) for i in range(2)]
            ya = [sbt(pe, "ya%d" % i, [128, D], F32) for i in range(2)]
            yb = [sbt(pe, "yb%d" % i, [128, D], F32) for i in range(2)]
            junk = sbt(pe, "junkE", [128, D], BF16)
            ss = [sbt(pe, "ssE%d" % i, [128, 1], F32) for i in range(2)]
            yo = [sbt(pe, "yo%d" % i, [128, D], F32) for i in range(2)]
            outb = Buf()
            for ob in range(NOB):
                i = ob % 2
                row = ob * 128
                dma("sync", lambda e: e.dma_start(out=x2[i].t[:], in_=x2_d[row:row + 128, :]), reads=[b_x2[ob]], writes=[x2[i].b])
                dma("gpsimd", lambda e: e.indirect_dma_start(out=ya[i].t[:, :], out_offset=None, in_=ys_d[:, :],
                                                             in_offset=bass.IndirectOffsetOnAxis(ap=slots.t[:, ob, 0:1], axis=0),
                                                             bounds_check=reg_nslot, oob_is_err=False),
                    reads=[slots.b], writes=[ya[i].b])
                dma("gpsimd", lambda e: e.indirect_dma_start(out=yb[i].t[:, :], out_offset=None, in_=ys_d[:, :],
                                                             in_offset=bass.IndirectOffsetOnAxis(ap=slots.t[:, ob, 1:2], axis=0),
                                                             bounds_check=reg_nslot, oob_is_err=False),
                    reads=[slots.b], writes=[yb[i].b])
                op("vector", lambda e: e.tensor_tensor(out=x2[i].t[:], in0=x2[i].t[:], in1=ya[i].t[:], op=ALU.add),
                   [x2[i].b, ya[i].b], [x2[i].b])
                op("gpsimd", lambda e: e.tensor_tensor(out=x2[i].t[:], in0=x2[i].t[:], in1=yb[i].t[:], op=ALU.add),
                   [x2[i].b, yb[i].b], [x2[i].b])
                rms_rstd(None, x2[i].t[:], x2[i].b, D, ss[i], junk)
                op("vector", lambda e: e.scalar_tensor_tensor(out=yo[i].t[:], in0=x2[i].t[:], scalar=ss[i].t[:, 0:1], in1=gfin.t[:],
                                                              op0=ALU.mult, op1=ALU.mult), [x2[i].b, ss[i].b, gfin.b], [yo[i].b])
                dma("sync", lambda e: e.dma_start(out=yout[row:row + 128, :], in_=yo[i].t[:]), reads=[yo[i].b], writes=[outb])
            Sx.barrier()
        print("built: ninst=%d nsem=%d" % (Sx.ninst, Sx.nsem))
    return nc


_CACHE = {}


def _diag_masks(r):
    s = np.arange(128)[:, None]
    t = np.arange(128)[None, :]
    strict = (s < t).astype(np.float32)
    m = np.zeros((4, 128, 128), np.float32)
    for k in range(4):
        if k < r:
            m[k] = 1.0
        elif k == r:
            m[k] = strict
    return m


def run(inputs, S, CB=3, debug=False, trace=False, stop=6):
    key = (S, CB, debug, stop)
    if key not in _CACHE:
        _CACHE[key] = build(S, CB, debug, stop)
    nc = _CACHE[key]
    x = np.asarray(inputs["x"], np.float32)
    mem = np.asarray(inputs["mem"], np.float32)
    NB = S // 128
    shared = {}
    for k_ in ("g_mix", "w_in", "g_gm", "w_spatial", "b_spatial", "w_branch_sb", "w_branch_gm", "w_out", "g_cross", "g_mem",
               "w_xq", "w_xkv", "w_xo", "g_ffn", "w_rg", "b_rg", "w_re", "b_re", "w_e_gate", "w_e_up", "w_e_down"):
        if stop < 5 and k_.startswith("w_e_"):
            continue
        shared[k_] = np.ascontiguousarray(np.asarray(inputs[k_], np.float32)[0])
    shared["g_final"] = np.ascontiguousarray(np.asarray(inputs["g_final"], np.float32))
    in_maps = []
    for c in range(8):
        b, r = c // 4, c % 4
        xbatch = np.ascontiguousarray(x[b])
        own = np.ascontiguousarray(xbatch.reshape(NB // 4, 4, 128, D)[:, r].reshape(-1, D))
        d = dict(shared)
        d["xb"] = xbatch
        d["xown"] = own
        d["memb"] = np.ascontiguousarray(mem[b])
        d["maskd"] = _diag_masks(r)
        in_maps.append(d)
    res = run_bass_kernel_spmd(nc, in_maps, core_ids=list(range(8)), trace=trace)
    out = np.empty((2, S, D), np.float32)
    for c in range(8):
        b, r = c // 4, c % 4
        y = np.asarray(res.results[c]["yout"]).reshape(NB // 4, 128, D)
        out[b].reshape(NB // 4, 4, 128, D)[:, r] = y
    return out, res


def kernel(**inputs):
    S = np.asarray(inputs["x"]).shape[1]
    out, _ = run(inputs, S)
    return out
```

```python
import math
from contextlib import ExitStack

import numpy as np
import concourse.bass as bass
import concourse.mybir as mybir
from concourse.bass_utils import run_bass_kernel_spmd
from concourse.alu_op_type import AluOpType as ALU

AF = mybir.ActivationFunctionType
F32 = mybir.dt.float32
BF16 = mybir.dt.bfloat16
I32 = mybir.dt.int32
AX = mybir.AxisListType

D = 2048
NE = 32
DFF = 1024
NMEM = 256
EPS = 1e-6


class Buf:
    __slots__ = ("w", "r", "x")

    def __init__(self, x=False):
        self.w = None
        self.r = {}
        self.x = x


class TT:
    __slots__ = ("t", "b")

    def __init__(self, t):
        self.t = t
        self.b = Buf()


class EngState:
    def __init__(self, name, eng):
        self.name = name
        self.eng = eng
        self.sem = None
        self.count = 0
        self.known = {}


class Sched:
    SEM_LIMIT = 30000
    N_DMA_SEMS = 12

    def __init__(self, nc, stack):
        self.nc = nc
        self.stack = stack
        self.sems = {}
        self.nsem = 0
        self.E = {}
        for name in ("tensor", "vector", "scalar", "gpsimd", "sync"):
            es = EngState(name, getattr(nc, name))
            self.E[name] = es
            self._new_eng_sem(es)
        self.dma_pool = {}
        for q in ("sync", "gpsimd", "scalar"):
            pool = []
            for i in range(self.N_DMA_SEMS):
                pool.append([self._alloc_sem("d_%s_%d" % (q, i)), 0])
            self.dma_pool[q] = [pool, 0]
        self.ninst = 0

    def _alloc_sem(self, name):
        h = self.stack.enter_context(self.nc.semaphore(name))
        key = self.nsem
        self.nsem += 1
        self.sems[key] = h
        return key

    def _new_eng_sem(self, es):
        es.sem = self._alloc_sem("e_%s_%d" % (es.name, self.nsem))
        es.count = 0

    def _wait(self, es, key, val):
        if val <= 0 or es.known.get(key, 0) >= val:
            return
        es.eng.wait_ge(self.sems[key], val)
        es.known[key] = val

    def _deps(self, es, reads, writes):
        toks = {}
        for b in reads:
            if b.w is not None:
                k, v = b.w
                if toks.get(k, 0) < v:
                    toks[k] = v
            if b.x:
                for k, v in b.r.items():
                    if k != es.sem and toks.get(k, 0) < v:
                        toks[k] = v
        for b in writes:
            if b.w is not None:
                k, v = b.w
                if toks.get(k, 0) < v:
                    toks[k] = v
            for k, v in b.r.items():
                if toks.get(k, 0) < v:
                    toks[k] = v
        for k, v in toks.items():
            if k == es.sem and es.name == "tensor":
                continue
            self._wait(es, k, v)

    def _commit(self, tok, reads, writes):
        k, v = tok
        for b in reads:
            if b.r.get(k, 0) < v:
                b.r[k] = v
        for b in writes:
            b.w = tok
            b.r = {}

    def op(self, eng, fn, reads=(), writes=()):
        es = self.E[eng]
        if es.count >= self.SEM_LIMIT:
            self._new_eng_sem(es)
        self._deps(es, reads, writes)
        inst = fn(es.eng)
        es.count += 1
        inst.then_inc(self.sems[es.sem], 1)
        self._commit((es.sem, es.count), reads, writes)
        self.ninst += 1
        return inst

    def dma(self, q, fn, reads=(), writes=()):
        es = self.E[q]
        self._deps(es, reads, writes)
        pool, idx = self.dma_pool[q]
        ent = pool[idx]
        self.dma_pool[q][1] = (idx + 1) % len(pool)
        self._wait(es, ent[0], ent[1])
        inst = fn(es.eng)
        ent[1] += 16
        inst.then_inc(self.sems[ent[0]], 16)
        self._commit((ent[0], ent[1]), reads, writes)
        self.ninst += 1
        return inst

    def barrier(self):
        targets = []
        for es in self.E.values():
            if es.count > 0:
                targets.append((es.sem, es.count))
        for q, (pool, _) in self.dma_pool.items():
            for key, val in pool:
                if val > 0:
                    targets.append((key, val))
        for es in self.E.values():
            for k, v in targets:
                if k == es.sem:
                    continue
                self._wait(es, k, v)


def build(S, CB=3, debug=False, stop=6):
    NB = S // 128
    NOB = NB // 4
    NCH = NOB // 4
    NTOK = NOB * 128
    NBC = NB // 4
    CAP = CB * 128
    NSLOT = NE * CAP
    ZROW = NTOK
    SCALE = 1.0 / math.sqrt(128.0)

    nc = bass.Bass("TRN2", target_bir_lowering=False)

    def din(name, shape, dt=F32):
        return nc.dram_tensor(name, list(shape), dt, kind="ExternalInput").ap()

    def dscr(name, shape, dt):
        return nc.dram_tensor(name, list(shape), dt, kind="ExternalOutput" if debug else "Internal").ap()

    xb = din("xb", [S, D])
    xown = din("xown", [NTOK, D])
    memb = din("memb", [NMEM, D])
    maskd = din("maskd", [4, 128, 128])
    g_mix = din("g_mix", [D]); w_in = din("w_in", [D, 9216]); g_gm = din("g_gm", [1024])
    w_sp = din("w_spatial", [8, 128, 128]); b_sp = din("b_spatial", [8, 128])
    w_bsb = din("w_branch_sb", [1024, D]); w_bgm = din("w_branch_gm", [1024, D]); w_out = din("w_out", [D, D])
    g_cross = din("g_cross", [D]); g_mem = din("g_mem", [D])
    w_xq = din("w_xq", [D, 512]); w_xkv = din("w_xkv", [D, 1024]); w_xo = din("w_xo", [512, D])
    g_ffn = din("g_ffn", [D]); w_rg = din("w_rg", [D, 4]); b_rg = din("b_rg", [4])
    w_re = din("w_re", [D, 32]); b_re = din("b_re", [32])
    if stop >= 5:
        w_eg = din("w_e_gate", [NE, D, DFF]); w_eu = din("w_e_up", [NE, D, DFF]); w_ed = din("w_e_down", [NE, DFF, D])
    g_final = din("g_final", [D])
    yout = nc.dram_tensor("yout", [NTOK, D], F32, kind="ExternalOutput").ap()

    kT_d = dscr("kT_d", [8, 128, S], BF16)
    v_d = dscr("v_d", [S, 1024], BF16)
    qT_d = dscr("qT_d", [8, 128, NTOK], BF16)
    oT_d = dscr("oT_d", [8, 128, NTOK], BF16)
    x2_d = dscr("x2_d", [NTOK, D], F32)
    hf_d = dscr("hf_d", [NTOK + 128, D], BF16)
    rec_d = dscr("rec_d", [NSLOT, 2], I32)
    ys_d = dscr("ys_d", [NSLOT, D], F32)
    b_kT = [[Buf() for _ in range(NBC)] for _ in range(8)]
    b_v = [Buf() for _ in range(NB)]
    b_qT = [[Buf() for _ in range(NCH)] for _ in range(8)]
    b_oT = [[Buf() for _ in range(NCH)] for _ in range(8)]
    b_x2 = [Buf() for _ in range(NOB)]
    b_hf = [Buf() for _ in range(NOB + 1)]
    b_rec = Buf()
    b_ys = [Buf() for _ in range(NE)]

    with ExitStack() as top:
        Sx = Sched(nc, top)
        op = Sx.op
        dma = Sx.dma

        def sbt(st, name, shape, dt):
            return TT(st.enter_context(nc.sbuf_tensor(name, list(shape), dt)))

        PSB = [TT(top.enter_context(nc.psum_tensor("psb%d" % i, [128, 512], F32))) for i in range(6)]
        for p_ in PSB:
            p_.b.x = True
        pst2 = [top.enter_context(nc.psum_tensor("pst%d" % i, [128, 8, 128], BF16)) for i in range(2)]

        class _PST:
            def __getitem__(self, key):
                p, idx, rest = key
                if isinstance(idx, slice):
                    hi = idx.start // 4
                    return pst2[hi][p, idx.start - hi * 4:idx.stop - hi * 4, rest]
                hi = idx // 4
                return pst2[hi][p, idx - hi * 4, rest]

        pst = _PST()
        PT_b = [Buf(True), Buf(True)]
        ps_rr = [0]

        def psn():
            p = PSB[ps_rr[0] % 6]
            ps_rr[0] += 1
            return p

        pt_rr = [0]

        def ptn():
            i = pt_rr[0] % 2
            pt_rr[0] += 1
            return i

        ev_rr = [0]

        def ev_eng():
            ev_rr[0] += 1
            return "vector" if ev_rr[0] % 2 else "scalar"

        def copy_on(eng, out, in_, reads, writes):
            if eng == "scalar":
                op("scalar", lambda e: e.activation(out=out, in_=in_, func=AF.Copy), reads, writes)
            else:
                op(eng, lambda e: e.tensor_copy(out=out, in_=in_), reads, writes)

        def mm(out, lhsT, rhs, start, stop, reads, writes, skip=False):
            op("tensor", lambda e: e.matmul(out, lhsT=lhsT, rhs=rhs, start=start, stop=stop, skip_group_check=skip), reads, writes)

        reg_nslot = nc.gpsimd.to_reg(NSLOT - 1)
        reg_ntok = nc.gpsimd.to_reg(NTOK + 127)
        cst = top
        identf = sbt(cst, "identf", [128, 128], F32)
        ident = sbt(cst, "ident", [128, 128], BF16)
        ones_bf = sbt(cst, "ones_bf", [128, 128], BF16)
        tri_ge = sbt(cst, "tri_ge", [128, 128], BF16)
        ustrict = sbt(cst, "ustrict", [128, 128], BF16)
        trilf = sbt(cst, "trilf", [128, 128], F32)
        gT = {}
        for nm, ap_ in (("mix", g_mix), ("cross", g_cross), ("mem", g_mem), ("ffn", g_ffn)):
            gT[nm] = sbt(cst, "gT_" + nm, [128, 16], F32)
            dma("sync", lambda e: e.dma_start(out=gT[nm].t[:], in_=ap_.rearrange("(c p) -> p c", p=128),
                                              allow_slow_non_contiguous=True), writes=[gT[nm].b])
        mk = sbt(cst, "mk", [128, 4, 128], F32)
        dma("sync", lambda e: e.dma_start(out=mk.t[:], in_=maskd.rearrange("k s t -> s k t")), writes=[mk.b])

        def tri_const(tt_f, cmp, cm, pat):
            op("gpsimd", lambda e: e.memset(tt_f.t[:], 1.0), writes=[tt_f.b])
            op("gpsimd", lambda e: e.affine_select(out=tt_f.t[:], in_=tt_f.t[:], pattern=[[pat, 128]],
                                                   compare_op=cmp, fill=0.0, base=0, channel_multiplier=cm),
               reads=[tt_f.b], writes=[tt_f.b])

        tri_const(identf, ALU.is_equal, 1, -1)
        op("vector", lambda e: e.tensor_copy(out=ident.t[:], in_=identf.t[:]), [identf.b], [ident.b])
        op("vector", lambda e: e.memset(ones_bf.t[:], 1.0), writes=[ones_bf.b])
        tri_const(trilf, ALU.is_ge, 1, -1)
        op("vector", lambda e: e.tensor_copy(out=tri_ge.t[:], in_=trilf.t[:]), [trilf.b], [tri_ge.b])
        tmpf = sbt(cst, "tmpf", [128, 128], F32)
        tri_const(tmpf, ALU.is_gt, -1, 1)
        op("vector", lambda e: e.tensor_copy(out=ustrict.t[:], in_=tmpf.t[:]), [tmpf.b], [ustrict.b])

        def rms_rstd(st_sc, x_ap, xbuf, width, ss, junk):
            op("scalar", lambda e: e.activation(out=junk.t[:, 0:width], in_=x_ap, func=AF.Square, accum_out=ss.t[:, 0:1]),
               [xbuf], [junk.b, ss.b])
            op("vector", lambda e: e.tensor_scalar(out=ss.t[:, 0:1], in0=ss.t[:, 0:1], scalar1=1.0 / width, scalar2=EPS,
                                                   op0=ALU.mult, op1=ALU.add), [ss.b], [ss.b])
            op("scalar", lambda e: e.activation(out=ss.t[:, 0:1], in_=ss.t[:, 0:1], func=AF.Ln), [ss.b], [ss.b])
            op("scalar", lambda e: e.activation(out=ss.t[:, 0:1], in_=ss.t[:, 0:1], func=AF.Exp, scale=-0.5), [ss.b], [ss.b])

        def norm_T(x_ap, xbuf, g_t, hT, col0, ss, junk, xn, ncols=128):
            rms_rstd(None, x_ap, xbuf, D, ss, junk)
            op("vector", lambda e: e.tensor_scalar(out=xn.t[:], in0=x_ap, scalar1=ss.t[:, 0:1], scalar2=None, op0=ALU.mult),
               [xbuf, ss.b], [xn.b])
            for cg in range(4):
                hi = ptn()
                for j in range(4):
                    c = cg * 4 + j
                    op("tensor", lambda e: e.transpose(out=pst[:, hi * 4 + j, 0:ncols], in_=xn.t[0:ncols, c * 128:(c + 1) * 128],
                                                       identity=ident.t[0:ncols, 0:ncols]),
                       [xn.b, ident.b], [PT_b[hi]])
                for j in range(4):
                    c = cg * 4 + j
                    eng = ev_eng()
                    if eng == "vector":
                        op("vector", lambda e: e.tensor_scalar(out=hT.t[:, c, col0:col0 + ncols], in0=pst[:, hi * 4 + j, 0:ncols],
                                                               scalar1=g_t.t[:, c:c + 1], scalar2=None, op0=ALU.mult),
                           [PT_b[hi], g_t.b], [hT.b])
                    else:
                        op("scalar", lambda e: e.activation(out=hT.t[:, c, col0:col0 + ncols], in_=pst[:, hi * 4 + j, 0:ncols],
                                                            func=AF.Copy, scale=g_t.t[:, c:c + 1]),
                           [PT_b[hi], g_t.b], [hT.b])

        NRING = 16
        ring = [sbt(top, "wr%d" % i, [128, 512], BF16) for i in range(NRING)]
        ring_i = [0]

        def wload(src_ap, ncols):
            t = ring[ring_i[0] % NRING]
            ring_i[0] += 1
            dma("gpsimd", lambda e: e.dma_start(out=t.t[:, 0:ncols], in_=src_ap), writes=[t.b])
            return t

        def linear_fm(xT, KC, N, w_ap, col0, ncols, evac):
            for c0 in range(0, ncols, 512):
                nog = min(4, (ncols - c0) // 128)
                banks = [psn() for _ in range(nog)]
                for c in range(KC):
                    wt = wload(w_ap[c * 128:(c + 1) * 128, col0 + c0:col0 + c0 + nog * 128], nog * 128)
                    for j in range(nog):
                        mm(banks[j].t[:, 0:N], wt.t[:, j * 128:(j + 1) * 128], xT.t[:, c, 0:N], c == 0, c == KC - 1,
                           [wt.b, xT.b], [banks[j].b])
                for j in range(nog):
                    evac(c0 // 128 + j, banks[j])

        def linear_tm(xT, KC, nblk, w_ap, col0, ncols, evac):
            for cg in range(ncols // 512):
                banks = [psn() for _ in range(nblk)]
                for c in range(KC):
                    wt = wload(w_ap[c * 128:(c + 1) * 128, col0 + cg * 512:col0 + cg * 512 + 512], 512)
                    for bl in range(nblk):
                        mm(banks[bl].t[:, :], xT.t[:, c, bl * 128:(bl + 1) * 128], wt.t[:, :], c == 0, c == KC - 1,
                           [wt.b, xT.b], [banks[bl].b])
                for bl in range(nblk):
                    evac(bl, cg, banks[bl])

        with ExitStack() as pa:
          if stop >= 1:
            wk = sbt(pa, "wk", [128, 16, 1024], BF16)
            wv = sbt(pa, "wv", [128, 16, 1024], BF16)
            wq = sbt(pa, "wq", [128, 16, 1024], BF16)
            for c in range(16):
                for tt_, cb in ((wq, 0), (wk, 1024), (wv, 2048)):
                    dma("gpsimd", lambda e: e.dma_start(out=tt_.t[:, c, :], in_=w_in[c * 128:(c + 1) * 128, cb:cb + 1024]),
                        writes=[tt_.b])
            xt = [sbt(pa, "xt%d" % i, [128, D], F32) for i in range(2)]
            junk = sbt(pa, "junkA", [128, D], BF16)
            xn = [sbt(pa, "xnA%d" % i, [128, D], BF16) for i in range(2)]
            ss = [sbt(pa, "ssA%d" % i, [128, 1], F32) for i in range(2)]
            hT = [sbt(pa, "hTA%d" % i, [128, 16, 512], BF16) for i in range(2)]
            ksb = [sbt(pa, "ksb%d" % i, [128, 512], BF16) for i in range(3)]
            vsb = [sbt(pa, "vsb%d" % i, [128, 1024], BF16) for i in range(2)]
            xi = 0
            ki = 0
            for ch in range(NBC):
                h_ = hT[ch % 2]
                for bl in range(4):
                    x_ = xt[xi % 2]; xn_ = xn[xi % 2]; ss_ = ss[xi % 2]; xi += 1
                    row = (ch * 4 + bl) * 128
                    dma("sync", lambda e: e.dma_start(out=x_.t[:], in_=xb[row:row + 128, :]), writes=[x_.b])
                    norm_T(x_.t[:], x_.b, gT["mix"], h_, bl * 128, ss_, junk, xn_)
                for h in range(8):
                    pk = psn()
                    for c in range(16):
                        mm(pk.t[:, :], wk.t[:, c, h * 128:(h + 1) * 128], h_.t[:, c, :], c == 0, c == 15, [wk.b, h_.b], [pk.b])
                    k_ = ksb[ki % 3]; ki += 1
                    copy_on(ev_eng(), k_.t[:], pk.t[:, :], [pk.b], [k_.b])
                    dma("sync", lambda e: e.dma_start(out=kT_d[h, :, ch * 512:(ch + 1) * 512], in_=k_.t[:]),
                        reads=[k_.b], writes=[b_kT[h][ch]])
                for bl in range(4):
                    v_ = vsb[bl % 2]
                    for cg in range(2):
                        pv = psn()
                        for c in range(16):
                            mm(pv.t[:, :], h_.t[:, c, bl * 128:(bl + 1) * 128], wv.t[:, c, cg * 512:(cg + 1) * 512], c == 0, c == 15,
                               [wv.b, h_.b], [pv.b])
                        copy_on(ev_eng(), v_.t[:, cg * 512:(cg + 1) * 512], pv.t[:, :], [pv.b], [v_.b])
                    row = (ch * 4 + bl) * 128
                    dma("sync", lambda e: e.dma_start(out=v_d[row:row + 128, :], in_=v_.t[:]), reads=[v_.b], writes=[b_v[ch * 4 + bl]])
            for m in range(NCH):
                h_ = hT[m % 2]
                for bl in range(4):
                    x_ = xt[xi % 2]; xn_ = xn[xi % 2]; ss_ = ss[xi % 2]; xi += 1
                    row = (m * 4 + bl) * 128
                    dma("sync", lambda e: e.dma_start(out=x_.t[:], in_=xown[row:row + 128, :]), writes=[x_.b])
                    norm_T(x_.t[:], x_.b, gT["mix"], h_, bl * 128, ss_, junk, xn_)
                for h in range(8):
                    pk = psn()
                    for c in range(16):
                        mm(pk.t[:, :], wq.t[:, c, h * 128:(h + 1) * 128], h_.t[:, c, :], c == 0, c == 15, [wq.b, h_.b], [pk.b])
                    k_ = ksb[ki % 3]; ki += 1
                    copy_on(ev_eng(), k_.t[:], pk.t[:, :], [pk.b], [k_.b])
                    dma("sync", lambda e: e.dma_start(out=qT_d[h, :, m * 512:(m + 1) * 512], in_=k_.t[:]),
                        reads=[k_.b], writes=[b_qT[h][m]])
            Sx.barrier()

        kxT = sbt(top, "kxT", [128, 4, NMEM], BF16)
        vx = sbt(top, "vx", [128, 2, 512], BF16)
        with ExitStack() as pm:
          if stop >= 2:
            xt = [sbt(pm, "xtM%d" % i, [128, D], F32) for i in range(2)]
            junk = sbt(pm, "junkM", [128, D], BF16)
            xn = [sbt(pm, "xnM%d" % i, [128, D], BF16) for i in range(2)]
            ss = [sbt(pm, "ssM%d" % i, [128, 1], F32) for i in range(2)]
            mT = sbt(pm, "mT", [128, 16, NMEM], BF16)
            for bl in range(2):
                dma("sync", lambda e: e.dma_start(out=xt[bl].t[:], in_=memb[bl * 128:(bl + 1) * 128, :]), writes=[xt[bl].b])
                norm_T(xt[bl].t[:], xt[bl].b, gT["mem"], mT, bl * 128, ss[bl], junk, xn[bl])

            def ev_kx(og, p):
                copy_on(ev_eng(), kxT.t[:, og, :], p.t[:, 0:NMEM], [p.b], [kxT.b])

            linear_fm(mT, 16, NMEM, w_xkv, 0, 512, ev_kx)

            def ev_vx(bl, cg, p):
                copy_on(ev_eng(), vx.t[:, bl, :], p.t[:, :], [p.b], [vx.b])

            linear_tm(mT, 16, 2, w_xkv, 512, 512, ev_vx)
            Sx.barrier()

        with ExitStack() as pb:
          if stop >= 3:
            kTh = [sbt(pb, "kTh%d" % i, [128, S], BF16) for i in range(2)]
            vh = [sbt(pb, "vh%d" % i, [128, NB, 128], BF16) for i in range(2)]
            qTh = [sbt(pb, "qTh%d" % i, [128, NTOK], BF16) for i in range(2)]
            NBUF = 4
            Et = [sbt(pb, "Et%d" % i, [128, 512], BF16) for i in range(NBUF)]
            spt = [sbt(pb, "spt%d" % i, [128, 512], BF16) for i in range(NBUF)]
            Gt = [sbt(pb, "Gt%d" % i, [128, 512], BF16) for i in range(NBUF)]
            At = [sbt(pb, "At%d" % i, [128, 512], BF16) for i in range(NBUF)]
            car = [sbt(pb, "car%d" % i, [128, 512], BF16) for i in range(NBUF)]
            mkb = sbt(pb, "mkb", [128, 4, 128], BF16)
            op("vector", lambda e: e.tensor_copy(out=mkb.t[:], in_=mk.t[:]), [mk.b], [mkb.b])
            sel0 = sbt(pb, "sel0", [128, 128], BF16)
            op("vector", lambda e: e.memset(sel0.t[:], 0.0), writes=[sel0.b])
            op("vector", lambda e: e.memset(sel0.t[0:1, :], 1.0), reads=[sel0.b], writes=[sel0.b])
            osb = [sbt(pb, "osb%d" % i, [128, 512], BF16) for i in range(2)]
            v_view = v_d.rearrange("(n p) (h d) -> h p n d", p=128, d=128)

            def load_head(h):
                i = h % 2
                dma("sync", lambda e: e.dma_start(out=kTh[i].t[:], in_=kT_d[h]), reads=[b for b in b_kT[h]], writes=[kTh[i].b])
                for n0 in range(0, NB, 8):
                    dma("sync", lambda e: e.dma_start(out=vh[i].t[:, n0:n0 + 8, :], in_=v_view[h][:, n0:n0 + 8, :]), reads=b_v, writes=[vh[i].b])
                dma("sync", lambda e: e.dma_start(out=qTh[i].t[:], in_=qT_d[h]), reads=[b for b in b_qT[h]], writes=[qTh[i].b])

            load_head(0)
            itc = [0]
            oi = 0
            for h in range(8):
                if h + 1 < 8:
                    load_head(h + 1)
                hi = h % 2
                for m in range(NCH):
                    po = PSB[oi % 2]
                    its = []
                    for kb in range(16 * m + 15, -1, -1):
                        if kb >= 16 * m:
                            its.append((kb, 128 * ((kb - 16 * m) // 4), (kb - 16 * m) % 4, True))
                        else:
                            its.append((kb, 0, 0, False))
                    base = itc[0]
                    itc[0] += len(its)

                    L = len(its)

                    def f_qk(n):
                        kb, c0, k, diag = its[n]
                        pS = PSB[2 + (base + n) % 2]
                        mm(pS.t[:, c0:512], kTh[hi].t[:, kb * 128:(kb + 1) * 128], qTh[hi].t[:, m * 512 + c0:(m + 1) * 512],
                           True, True, [kTh[hi].b, qTh[hi].b], [pS.b])

                    def f_e(n):
                        kb, c0, k, diag = its[n]
                        i = (base + n) % NBUF
                        pS = PSB[2 + (base + n) % 2]
                        op("scalar", lambda e: e.activation(out=Et[i].t[:, c0:512], in_=pS.t[:, c0:512], func=AF.Exp, scale=SCALE),
                           [pS.b], [Et[i].b])
                        if diag:
                            op("vector", lambda e: e.tensor_tensor(out=Et[i].t[:, c0:c0 + 128], in0=Et[i].t[:, c0:c0 + 128],
                                                                  in1=mkb.t[:, k, :], op=ALU.mult), [Et[i].b, mkb.b], [Et[i].b])
                        op("scalar", lambda e: e.activation(out=spt[i].t[:, c0:512], in_=Et[i].t[:, c0:512], func=AF.Ln, bias=1.0),
                           [Et[i].b], [spt[i].b])

                    def f_m(n):
                        kb, c0, k, diag = its[n]
                        i = (base + n) % NBUF
                        pC = PSB[4 + (base + n) % 2]
                        has_carry = n > 0
                        mm(pC.t[:, c0:512], tri_ge.t[:, :], spt[i].t[:, c0:512], True, not has_carry, [tri_ge.b, spt[i].b], [pC.b], skip=True)
                        if has_carry:
                            pc0 = its[n - 1][1]
                            ip = (base + n - 1) % NBUF
                            mm(pC.t[:, pc0:512], sel0.t[:, :], car[ip].t[:, pc0:512], False, True, [sel0.b, car[ip].b], [pC.b], skip=True)
                        if n + 1 < L:
                            op("vector", lambda e: e.tensor_copy(out=car[i].t[:, c0:512], in_=pC.t[:, c0:512]), [pC.b], [car[i].b])

                    def f_g(n):
                        kb, c0, k, diag = its[n]
                        i = (base + n) % NBUF
                        pC = PSB[4 + (base + n) % 2]
                        op("scalar", lambda e: e.activation(out=Gt[i].t[:, c0:512], in_=pC.t[:, c0:512], func=AF.Exp, scale=-1.0),
                           [pC.b], [Gt[i].b])

                    def f_a(n):
                        kb, c0, k, diag = its[n]
                        i = (base + n) % NBUF
                        op("vector", lambda e: e.tensor_tensor(out=At[i].t[:, c0:512], in0=Et[i].t[:, c0:512], in1=Gt[i].t[:, c0:512],
                                                              op=ALU.mult), [Et[i].b, Gt[i].b], [At[i].b])
                        mm(po.t[:, c0:512], vh[hi].t[:, kb, :], At[i].t[:, c0:512], n == 0, n == L - 1, [vh[hi].b, At[i].b], [po.b], skip=True)

                    for t in range(-3, L + 1):
                        for fn, lag in ((f_qk, 3), (f_e, 2), (f_m, 1), (f_g, 0), (f_a, -1)):
                            n = t + lag
                            if 0 <= n < L:
                                fn(n)
                    o_ = osb[oi % 2]; oi += 1
                    op("vector", lambda e: e.tensor_copy(out=o_.t[:], in_=po.t[:, :]), [po.b], [o_.b])
                    dma("sync", lambda e: e.dma_start(out=oT_d[h, :, m * 512:(m + 1) * 512], in_=o_.t[:]),
                        reads=[o_.b], writes=[b_oT[h][m]])
            Sx.barrier()

        slots = sbt(top, "slots", [128, NOB, 2], I32)
        with ExitStack() as pc:
          if stop >= 4:
            xres = sbt(pc, "xres", [128, 4, D], F32)
            hT = sbt(pc, "hTC", [128, 16, 512], BF16)
            junk = sbt(pc, "junkC", [128, D], BF16)
            xn = [sbt(pc, "xnC%d" % i, [128, D], BF16) for i in range(2)]
            ss = [sbt(pc, "ssC%d" % i, [128, 1], F32) for i in range(2)]
            uT = sbt(pc, "uT", [128, 8, 512], BF16)
            vtm = [sbt(pc, "vtm%d" % i, [128, 1024], F32) for i in range(2)]
            vnb = [sbt(pc, "vnb%d" % i, [128, 1024], BF16) for i in range(2)]
            ogT = sbt(pc, "ogT", [128, 8, 512], BF16)
            oT = sbt(pc, "oT", [128, 8, 512], BF16)
            mA = sbt(pc, "mA", [128, 16, 512], BF16)
            mB = sbt(pc, "mB", [128, 16, 512], BF16)
            sg = [sbt(pc, "sg%d" % i, [128, 512], F32) for i in range(2)]
            ggm_bc = sbt(pc, "ggm_bc", [128, 1024], F32)
            bias_rep = sbt(pc, "bias_rep", [128, 8, 128], F32)
            wmT = sbt(pc, "wmT", [128, 8, 128], BF16)
            qxT = sbt(pc, "qxT", [128, 4, 512], BF16)
            oxT = sbt(pc, "oxT", [128, 4, 512], BF16)
            Pf = sbt(pc, "Pf", [128, 4, NMEM], F32)
            Pn = sbt(pc, "Pn", [128, 4, NMEM], BF16)
            PTs = sbt(pc, "PTs", [128, 8, 128], BF16)
            sm = sbt(pc, "sm", [128, 16], F32)
            wr = sbt(pc, "wr", [128, 16, 36], BF16)
            wrf = sbt(pc, "wrf", [128, 16, 36], F32)
            rb = sbt(pc, "rb", [128, 36], F32)
            eoff = sbt(pc, "eoff", [128, 32], F32)
            tokf = sbt(pc, "tokf", [128, 1], F32)
            cum = sbt(pc, "cum", [128, 32], F32)
            cum_bf = sbt(pc, "cum_bf", [128, 32], BF16)
            R = sbt(pc, "R", [128, 200], F32)
            msk_bf = sbt(pc, "msk_bf", [128, 32], BF16)
            rec = [sbt(pc, "rec%d" % i, [128, 2], I32) for i in range(4)]
            sl_i = [sbt(pc, "sl_i%d" % i, [128, 1], I32) for i in range(4)]
            wsp_f = TT(vtm[0].t.rearrange("p (g s) -> p g s", g=8)); wsp_f.b = vtm[0].b
            wsp_b = TT(vnb[0].t.rearrange("p (g s) -> p g s", g=8)); wsp_b.b = vnb[0].b
            zrow = junk
            reci = sbt(pc, "reci", [128, NSLOT // 128, 2], I32)

            dma("sync", lambda e: e.dma_start(out=ggm_bc.t[:], in_=g_gm.partition_broadcast(128)), writes=[ggm_bc.b])
            dma("sync", lambda e: e.dma_start(out=bias_rep.t[:], in_=b_sp.partition_broadcast(128)), writes=[bias_rep.b])
            dma("sync", lambda e: e.dma_start(out=wsp_f.t[:], in_=w_sp.rearrange("g t s -> t g s")), writes=[wsp_f.b])
            for g in range(8):
                op("gpsimd", lambda e: e.tensor_tensor(out=wsp_f.t[:, g, :], in0=wsp_f.t[:, g, :], in1=trilf.t[:, :], op=ALU.mult),
                   [wsp_f.b, trilf.b], [wsp_f.b])
            op("vector", lambda e: e.tensor_copy(out=wsp_b.t[:], in_=wsp_f.t[:]), [wsp_f.b], [wsp_b.b])
            for g in range(8):
                hi = ptn()
                op("tensor", lambda e: e.transpose(out=pst[:, hi * 4, :], in_=wsp_b.t[:, g, :], identity=ident.t[:, :]),
                   [wsp_b.b, ident.b], [PT_b[hi]])
                op("vector", lambda e: e.tensor_copy(out=wmT.t[:, g, :], in_=pst[:, hi * 4, :]), [PT_b[hi]], [wmT.b])
            dma("sync", lambda e: e.dma_start(out=wrf.t[:, :, 0:4], in_=w_rg.rearrange("(c p) n -> p c n", p=128)), writes=[wrf.b])
            dma("sync", lambda e: e.dma_start(out=wrf.t[:, :, 4:36], in_=w_re.rearrange("(c p) n -> p c n", p=128)), writes=[wrf.b])
            op("vector", lambda e: e.tensor_copy(out=wr.t[:], in_=wrf.t[:]), [wrf.b], [wr.b])
            dma("sync", lambda e: e.dma_start(out=rb.t[:, 0:4], in_=b_rg.partition_broadcast(128)), writes=[rb.b])
            dma("sync", lambda e: e.dma_start(out=rb.t[:, 4:36], in_=b_re.partition_broadcast(128)), writes=[rb.b])
            op("gpsimd", lambda e: e.iota(eoff.t[:], pattern=[[CAP, 32]], base=0, channel_multiplier=0,
                                          allow_small_or_imprecise_dtypes=True), writes=[eoff.b])
            op("gpsimd", lambda e: e.iota(tokf.t[:], pattern=[[0, 1]], base=0, channel_multiplier=1,
                                          allow_small_or_imprecise_dtypes=True), writes=[tokf.b])
            op("vector", lambda e: e.memset(cum.t[:], 0.0), writes=[cum.b])
            op("vector", lambda e: e.memset(zrow.t[:], 0.0), writes=[zrow.b])
            dma("sync", lambda e: e.dma_start(out=hf_d[NTOK:NTOK + 128, :], in_=zrow.t[:]), reads=[zrow.b], writes=[b_hf[NOB]])
            op("vector", lambda e: e.memset(reci.t[:, :, 0:1], ZROW), writes=[reci.b])
            op("vector", lambda e: e.memset(reci.t[:, :, 1:2], 0), writes=[reci.b])
            dma("sync", lambda e: e.dma_start(out=rec_d.rearrange("(p n) c -> p n c", p=128), in_=reci.t[:]),
                reads=[reci.b], writes=[b_rec])

            ri = 0
            for m in range(NCH):
                for bl in range(4):
                    row = (m * 4 + bl) * 128
                    dma("sync", lambda e: e.dma_start(out=xres.t[:, bl, :], in_=xown[row:row + 128, :]), writes=[xres.b])
                dma("sync", lambda e: e.dma_start(out=oT.t[:], in_=oT_d[:, :, m * 512:(m + 1) * 512].rearrange("h p n -> p h n")),
                    reads=[b_oT[h][m] for h in range(8)], writes=[oT.b])
                for bl in range(4):
                    norm_T(xres.t[:, bl, :], xres.b, gT["mix"], hT, bl * 128, ss[bl % 2], junk, xn[bl % 2])

                def ev_u(og, p):
                    op("scalar", lambda e: e.activation(out=uT.t[:, og, :], in_=p.t[:, :], func=AF.Gelu), [p.b], [uT.b])

                linear_fm(hT, 16, 512, w_in, 3072, 1024, ev_u)

                def ev_v(bl, cg, p):
                    v_ = vtm[bl % 2]
                    op("scalar", lambda e: e.activation(out=v_.t[:, cg * 512:(cg + 1) * 512], in_=p.t[:, :], func=AF.Gelu),
                       [p.b], [v_.b])
                    if cg == 1:
                        s_ = ss[bl % 2]
                        rms_rstd(None, v_.t[:], v_.b, 1024, s_, junk)
                        op("vector", lambda e: e.scalar_tensor_tensor(out=vnb[bl % 2].t[:], in0=v_.t[:], scalar=s_.t[:, 0:1],
                                                                      in1=ggm_bc.t[:], op0=ALU.mult, op1=ALU.mult),
                           [v_.b, s_.b, ggm_bc.b], [vnb[bl % 2].b])

                for bl in range(4):
                    for cg in range(2):
                        pbank = psn()
                        for c in range(16):
                            wt = wload(w_in[c * 128:(c + 1) * 128, 4096 + cg * 512:4096 + cg * 512 + 512], 512)
                            mm(pbank.t[:, :], hT.t[:, c, bl * 128:(bl + 1) * 128], wt.t[:, :], c == 0, c == 15, [wt.b, hT.b], [pbank.b])
                        ev_v(bl, cg, pbank)
                    for gh in range(2):
                        pm_ = psn()
                        for gg in range(4):
                            g = gh * 4 + gg
                            mm(pm_.t[:, gg * 128:(gg + 1) * 128], vnb[bl % 2].t[:, g * 128:(g + 1) * 128], wmT.t[:, g, :], True, True,
                               [vnb[bl % 2].b, wmT.b], [pm_.b])
                        for gg in range(4):
                            g = gh * 4 + gg
                            s_ = sg[(g + bl) % 2]
                            op("vector", lambda e: e.tensor_tensor(out=s_.t[:, 0:128], in0=pm_.t[:, gg * 128:(gg + 1) * 128],
                                                                  in1=bias_rep.t[:, g, 0:128], op=ALU.add), [pm_.b, bias_rep.b], [s_.b])
                            op("gpsimd", lambda e: e.tensor_tensor(out=ogT.t[:, g, bl * 128:(bl + 1) * 128], in0=s_.t[:, 0:128],
                                                                  in1=uT.t[:, g, bl * 128:(bl + 1) * 128], op=ALU.mult),
                               [s_.b, uT.b], [ogT.b])

                def ev_store(dst):
                    def f(og, p):
                        copy_on(ev_eng(), dst.t[:, og, :], p.t[:, :], [p.b], [dst.b])
                    return f

                def ev_gate(dst):
                    def f(og, p):
                        s_ = sg[og % 2]
                        op("scalar", lambda e: e.activation(out=s_.t[:, :], in_=p.t[:, :], func=AF.Sigmoid), [p.b], [s_.b])
                        op("gpsimd", lambda e: e.tensor_tensor(out=dst.t[:, og, :], in0=dst.t[:, og, :], in1=s_.t[:, :], op=ALU.mult),
                           [s_.b, dst.b], [dst.b])
                    return f

                linear_fm(oT, 8, 512, w_bsb, 0, D, ev_store(mA))
                linear_fm(hT, 16, 512, w_in, 5120, D, ev_gate(mA))
                linear_fm(ogT, 8, 512, w_bgm, 0, D, ev_store(mB))
                linear_fm(hT, 16, 512, w_in, 7168, D, ev_gate(mB))
                for og in range(16):
                    op("vector" if og % 2 else "gpsimd",
                       lambda e: e.tensor_tensor(out=mA.t[:, og, :], in0=mA.t[:, og, :], in1=mB.t[:, og, :], op=ALU.add),
                       [mA.b, mB.b], [mA.b])

                def ev_res(bl, cg, p):
                    op("vector", lambda e: e.tensor_tensor(out=xres.t[:, bl, cg * 512:(cg + 1) * 512], in0=p.t[:, :],
                                                          in1=xres.t[:, bl, cg * 512:(cg + 1) * 512], op=ALU.add),
                       [p.b, xres.b], [xres.b])

                linear_tm(mA, 16, 4, w_out, 0, D, ev_res)

                for bl in range(4):
                    norm_T(xres.t[:, bl, :], xres.b, gT["cross"], hT, bl * 128, ss[bl % 2], junk, xn[bl % 2])
                linear_fm(hT, 16, 512, w_xq, 0, 512, ev_store(qxT))
                XS = 1.0 / math.sqrt(128.0)
                for bl in range(4):
                    pS2 = [psn(), psn()]
                    for hh in range(4):
                        p_ = pS2[hh // 2]
                        mm(p_.t[:, (hh % 2) * 256:(hh % 2) * 256 + 256], qxT.t[:, hh, bl * 128:(bl + 1) * 128], kxT.t[:, hh, :],
                           True, True, [qxT.b, kxT.b], [p_.b])
                    for hh in range(4):
                        p_ = pS2[hh // 2]
                        op("vector", lambda e: e.reduce_max(out=sm.t[:, hh:hh + 1], in_=p_.t[:, (hh % 2) * 256:(hh % 2) * 256 + 256],
                                                           axis=AX.X), [p_.b], [sm.b])
                    op("vector", lambda e: e.tensor_scalar(out=sm.t[:, 4:8], in0=sm.t[:, 0:4], scalar1=-XS, scalar2=None, op0=ALU.mult),
                       [sm.b], [sm.b])
                    for hh in range(4):
                        p_ = pS2[hh // 2]
                        op("scalar", lambda e: e.activation(out=Pf.t[:, hh, :], in_=p_.t[:, (hh % 2) * 256:(hh % 2) * 256 + 256],
                                                            func=AF.Exp, bias=sm.t[:, 4 + hh:5 + hh], scale=XS,
                                                            accum_out=sm.t[:, 8 + hh:9 + hh]), [p_.b, sm.b], [Pf.b, sm.b])
                    op("vector", lambda e: e.reciprocal(out=sm.t[:, 12:16], in_=sm.t[:, 8:12]), [sm.b], [sm.b])
                    for hh in range(4):
                        op("vector", lambda e: e.tensor_scalar(out=Pn.t[:, hh, :], in0=Pf.t[:, hh, :], scalar1=sm.t[:, 12 + hh:13 + hh],
                                                               scalar2=None, op0=ALU.mult), [Pf.b, sm.b], [Pn.b])
                    for half in range(2):
                        hi = ptn()
                        for j in range(4):
                            hh = half * 2 + j // 2
                            mc = j % 2
                            op("tensor", lambda e: e.transpose(out=pst[:, hi * 4 + j, :], in_=Pn.t[:, hh, mc * 128:(mc + 1) * 128],
                                                               identity=ident.t[:, :]), [Pn.b, ident.b], [PT_b[hi]])
                        copy_on(ev_eng(), PTs.t[:, half * 4:half * 4 + 4, :], pst[:, hi * 4:hi * 4 + 4, :], [PT_b[hi]], [PTs.b])
                    pO = psn()
                    for hh in range(4):
                        for mc in range(2):
                            mm(pO.t[:, hh * 128:(hh + 1) * 128], vx.t[:, mc, hh * 128:(hh + 1) * 128], PTs.t[:, hh * 2 + mc, :],
                               mc == 0, mc == 1, [vx.b, PTs.b], [pO.b])
                    copy_on(ev_eng(), oxT.t[:, :, bl * 128:(bl + 1) * 128], pO.t[:, :].rearrange("p (h n) -> p h n", h=4),
                            [pO.b], [oxT.b])
                linear_tm(oxT, 4, 4, w_xo, 0, D, ev_res)

                for bl in range(4):
                    ob = m * 4 + bl
                    row = ob * 128
                    xn_ = xn[bl % 2]
                    norm_T(xres.t[:, bl, :], xres.b, gT["ffn"], hT, bl * 128, ss[bl % 2], junk, xn_)
                    dma("sync", lambda e: e.dma_start(out=x2_d[row:row + 128, :], in_=xres.t[:, bl, :]), reads=[xres.b], writes=[b_x2[ob]])
                    dma("sync", lambda e: e.dma_start(out=hf_d[row:row + 128, :], in_=xn_.t[:]), reads=[xn_.b], writes=[b_hf[ob]])
                    pl = psn()
                    for c in range(16):
                        mm(pl.t[:, 0:36], hT.t[:, c, bl * 128:(bl + 1) * 128], wr.t[:, c, :], c == 0, c == 15, [hT.b, wr.b], [pl.b])
                    Rt = R.t
                    LG, GM, OHG, EL, M1, OH1, EL2, M2, OH2, P8 = 0, 36, 40, 44, 52, 56, 64, 72, 76, 84
                    NGM, SUMG, PG, DD, W1, W2, OH12, M32, POS, SL1, SL2, TOK = 92, 93, 94, 95, 96, 97, 98, 106, 138, 150, 151, 152
                    rbuf = [R.b]

                    def V(fn, extra_r=(), extra_w=()):
                        op("vector", fn, rbuf + list(extra_r), rbuf + list(extra_w))

                    V(lambda e: e.tensor_tensor(out=Rt[:, LG:LG + 36], in0=pl.t[:, 0:36], in1=rb.t[:, :], op=ALU.add), [pl.b, rb.b])
                    V(lambda e: e.reduce_max(out=Rt[:, GM:GM + 1], in_=Rt[:, LG:LG + 4], axis=AX.X))
                    V(lambda e: e.tensor_scalar(out=Rt[:, OHG:OHG + 4], in0=Rt[:, LG:LG + 4], scalar1=Rt[:, GM:GM + 1], scalar2=None,
                                                op0=ALU.is_equal))
                    V(lambda e: e.tensor_scalar(out=Rt[:, NGM:NGM + 1], in0=Rt[:, GM:GM + 1], scalar1=-1.0, scalar2=None, op0=ALU.mult))
                    op("scalar", lambda e: e.activation(out=Rt[:, P8:P8 + 4], in_=Rt[:, LG:LG + 4], func=AF.Exp, bias=Rt[:, NGM:NGM + 1],
                                                        scale=1.0, accum_out=Rt[:, SUMG:SUMG + 1]), rbuf, rbuf)
                    V(lambda e: e.reciprocal(out=Rt[:, PG:PG + 1], in_=Rt[:, SUMG:SUMG + 1]))
                    V(lambda e: e.tensor_scalar(out=Rt[:, EL:EL + 8], in0=Rt[:, LG + 4:LG + 12], scalar1=Rt[:, OHG:OHG + 1], scalar2=None,
                                                op0=ALU.mult))
                    for g in range(1, 4):
                        V(lambda e: e.scalar_tensor_tensor(out=Rt[:, EL:EL + 8], in0=Rt[:, LG + 4 + 8 * g:LG + 12 + 8 * g],
                                                           scalar=Rt[:, OHG + g:OHG + g + 1], in1=Rt[:, EL:EL + 8],
                                                           op0=ALU.mult, op1=ALU.add))
                    V(lambda e: e.reduce_max(out=Rt[:, M1:M1 + 1], in_=Rt[:, EL:EL + 8], axis=AX.X))
                    V(lambda e: e.tensor_scalar(out=Rt[:, OH1:OH1 + 8], in0=Rt[:, EL:EL + 8], scalar1=Rt[:, M1:M1 + 1], scalar2=None,
                                                op0=ALU.is_equal))
                    V(lambda e: e.scalar_tensor_tensor(out=Rt[:, EL2:EL2 + 8], in0=Rt[:, OH1:OH1 + 8], scalar=-1e30,
                                                       in1=Rt[:, EL:EL + 8], op0=ALU.mult, op1=ALU.add))
                    V(lambda e: e.reduce_max(out=Rt[:, M2:M2 + 1], in_=Rt[:, EL2:EL2 + 8], axis=AX.X))
                    V(lambda e: e.tensor_scalar(out=Rt[:, OH2:OH2 + 8], in0=Rt[:, EL2:EL2 + 8], scalar1=Rt[:, M2:M2 + 1], scalar2=None,
                                                op0=ALU.is_equal))
                    V(lambda e: e.tensor_tensor(out=Rt[:, DD:DD + 1], in0=Rt[:, M2:M2 + 1], in1=Rt[:, M1:M1 + 1], op=ALU.subtract))
                    op("scalar", lambda e: e.activation(out=Rt[:, DD:DD + 1], in_=Rt[:, DD:DD + 1], func=AF.Exp), rbuf, rbuf)
                    V(lambda e: e.tensor_scalar(out=Rt[:, DD:DD + 1], in0=Rt[:, DD:DD + 1], scalar1=1.0, scalar2=None, op0=ALU.add))
                    V(lambda e: e.reciprocal(out=Rt[:, W1:W1 + 1], in_=Rt[:, DD:DD + 1]))
                    V(lambda e: e.tensor_tensor(out=Rt[:, W1:W1 + 1], in0=Rt[:, W1:W1 + 1], in1=Rt[:, PG:PG + 1], op=ALU.mult))
                    V(lambda e: e.tensor_tensor(out=Rt[:, W2:W2 + 1], in0=Rt[:, PG:PG + 1], in1=Rt[:, W1:W1 + 1], op=ALU.subtract))
                    V(lambda e: e.tensor_tensor(out=Rt[:, OH12:OH12 + 8], in0=Rt[:, OH1:OH1 + 8], in1=Rt[:, OH2:OH2 + 8], op=ALU.add))
                    for g in range(4):
                        V(lambda e: e.tensor_scalar(out=Rt[:, M32 + 8 * g:M32 + 8 * g + 8], in0=Rt[:, OH12:OH12 + 8],
                                                    scalar1=Rt[:, OHG + g:OHG + g + 1], scalar2=None, op0=ALU.mult))
                    op("vector", lambda e: e.tensor_copy(out=msk_bf.t[:], in_=Rt[:, M32:M32 + 32]), rbuf, [msk_bf.b])
                    pp = psn()
                    mm(pp.t[:, 0:32], ustrict.t[:, :], msk_bf.t[:, :], True, ob == 0, [ustrict.b, msk_bf.b], [pp.b])
                    if ob > 0:
                        mm(pp.t[:, 0:32], ones_bf.t[:, :], cum_bf.t[:, :], False, True, [ones_bf.b, cum_bf.b], [pp.b])
                    PC = 160
                    V(lambda e: e.tensor_scalar(out=Rt[:, PC:PC + 32], in0=pp.t[:, 0:32], scalar1=float(CAP - 1), scalar2=None, op0=ALU.min),
                      [pp.b])
                    V(lambda e: e.tensor_tensor(out=Rt[:, PC:PC + 32], in0=Rt[:, PC:PC + 32], in1=eoff.t[:, :], op=ALU.add), [eoff.b])
                    op("vector", lambda e: e.tensor_tensor(out=cum.t[:], in0=cum.t[:], in1=Rt[:, M32:M32 + 32], op=ALU.add),
                       rbuf + [cum.b], [cum.b])
                    op("vector", lambda e: e.tensor_copy(out=cum_bf.t[:], in_=cum.t[:]), [cum.b], [cum_bf.b])
                    V(lambda e: e.tensor_scalar(out=Rt[:, P8:P8 + 8], in0=Rt[:, PC:PC + 8], scalar1=Rt[:, OHG:OHG + 1], scalar2=None,
                                                op0=ALU.mult))
                    for g in range(1, 4):
                        V(lambda e: e.scalar_tensor_tensor(out=Rt[:, P8:P8 + 8], in0=Rt[:, PC + 8 * g:PC + 8 * g + 8],
                                                           scalar=Rt[:, OHG + g:OHG + g + 1], in1=Rt[:, P8:P8 + 8],
                                                           op0=ALU.mult, op1=ALU.add))
                    V(lambda e: e.tensor_tensor(out=Rt[:, EL:EL + 8], in0=Rt[:, OH1:OH1 + 8], in1=Rt[:, P8:P8 + 8], op=ALU.mult))
                    V(lambda e: e.reduce_sum(out=Rt[:, SL1:SL1 + 1], in_=Rt[:, EL:EL + 8], axis=AX.X))
                    V(lambda e: e.tensor_tensor(out=Rt[:, EL2:EL2 + 8], in0=Rt[:, OH2:OH2 + 8], in1=Rt[:, P8:P8 + 8], op=ALU.mult))
                    V(lambda e: e.reduce_sum(out=Rt[:, SL2:SL2 + 1], in_=Rt[:, EL2:EL2 + 8], axis=AX.X))
                    V(lambda e: e.tensor_scalar(out=Rt[:, TOK:TOK + 1], in0=tokf.t[:, 0:1], scalar1=float(row), scalar2=None, op0=ALU.add),
                      [tokf.b])
                    op("vector", lambda e: e.tensor_copy(out=slots.t[:, ob, 0:1], in_=Rt[:, SL1:SL1 + 1]), rbuf, [slots.b])
                    op("vector", lambda e: e.tensor_copy(out=slots.t[:, ob, 1:2], in_=Rt[:, SL2:SL2 + 1]), rbuf, [slots.b])
                    for ch_, (slc, wc) in enumerate(((SL1, W1), (SL2, W2))):
                        rc = rec[ri % 4]; si = sl_i[ri % 4]; ri += 1
                        rcf = rc.t.bitcast(F32)
                        op("vector", lambda e: e.tensor_copy(out=rc.t[:, 0:1], in_=Rt[:, TOK:TOK + 1]), rbuf, [rc.b])
                        op("vector", lambda e: e.tensor_copy(out=rcf[:, 1:2], in_=Rt[:, wc:wc + 1]), rbuf, [rc.b])
                        op("vector", lambda e: e.tensor_copy(out=si.t[:, 0:1], in_=Rt[:, slc:slc + 1]), rbuf, [si.b])
                        dma("gpsimd", lambda e: e.indirect_dma_start(out=rec_d[:, :],
                                                                     out_offset=bass.IndirectOffsetOnAxis(ap=si.t[:, 0:1], axis=0),
                                                                     in_=rc.t[:, :], in_offset=None,
                                                                     bounds_check=reg_nslot, oob_is_err=False),
                            reads=[rc.b, si.b], writes=[b_rec])
            Sx.barrier()

        with ExitStack() as pd:
          if stop >= 5:
            idx = [sbt(pd, "idx%d" % i, [128, CB, 2], I32) for i in range(2)]
            xg = [sbt(pd, "xg%d" % i, [128, D], BF16) for i in range(3)]
            xT = [sbt(pd, "xTD%d" % i, [128, 16, CAP], BF16) for i in range(2)]
            aT = [sbt(pd, "aT%d" % i, [128, 8, CAP], BF16) for i in range(2)]
            sl = [sbt(pd, "sl%d" % i, [128, CAP], F32) for i in range(4)]
            ysb = [sbt(pd, "ysb%d" % i, [128, 512], F32) for i in range(4)]
            sli = 0
            yi = 0
            gi = 0
            for ex in range(NE):
                id_ = idx[ex % 2]
                x_T = xT[ex % 2]
                a_T = aT[ex % 2]
                dma("sync", lambda e: e.dma_start(out=id_.t[:], in_=rec_d[ex * CAP:(ex + 1) * CAP, :].rearrange("(b p) c -> p b c", p=128)),
                    reads=[b_rec], writes=[id_.b])
                for bl in range(CB):
                    g_ = xg[gi % 3]; gi += 1
                    dma("gpsimd", lambda e: e.indirect_dma_start(out=g_.t[:, :], out_offset=None, in_=hf_d[:, :],
                                                                 in_offset=bass.IndirectOffsetOnAxis(ap=id_.t[:, bl, 0:1], axis=0),
                                                                 bounds_check=reg_ntok, oob_is_err=False),
                        reads=[id_.b] + b_hf, writes=[g_.b])
                    for cg in range(4):
                        hi = ptn()
                        for j in range(4):
                            c = cg * 4 + j
                            op("tensor", lambda e: e.transpose(out=pst[:, hi * 4 + j, :], in_=g_.t[:, c * 128:(c + 1) * 128],
                                                               identity=ident.t[:, :]), [g_.b, ident.b], [PT_b[hi]])
                        for j in range(4):
                            c = cg * 4 + j
                            eng = ev_eng()
                            if eng == "vector":
                                op("vector", lambda e: e.tensor_scalar(out=x_T.t[:, c, bl * 128:(bl + 1) * 128], in0=pst[:, hi * 4 + j, :],
                                                                       scalar1=gT["ffn"].t[:, c:c + 1], scalar2=None, op0=ALU.mult),
                                   [PT_b[hi], gT["ffn"].b], [x_T.b])
                            else:
                                op("scalar", lambda e: e.activation(out=x_T.t[:, c, bl * 128:(bl + 1) * 128], in_=pst[:, hi * 4 + j, :],
                                                                    func=AF.Copy, scale=gT["ffn"].t[:, c:c + 1]),
                                   [PT_b[hi], gT["ffn"].b], [x_T.b])
                for fg in range(2):
                    silus = []
                    banks1 = [psn() for _ in range(4)]
                    for c in range(16):
                        wt = wload(w_eg[ex, c * 128:(c + 1) * 128, fg * 512:(fg + 1) * 512], 512)
                        for j in range(4):
                            mm(banks1[j].t[:, 0:CAP], wt.t[:, j * 128:(j + 1) * 128], x_T.t[:, c, :], c == 0, c == 15, [wt.b, x_T.b], [banks1[j].b])
                    for j in range(4):
                        s_ = sl[sli % 4]; sli += 1
                        op("scalar", lambda e: e.activation(out=s_.t[:, :], in_=banks1[j].t[:, 0:CAP], func=AF.Silu), [banks1[j].b], [s_.b])
                        silus.append(s_)
                    banks2 = [psn(), psn(), banks1[0], banks1[1]]
                    for c in range(16):
                        wt = wload(w_eu[ex, c * 128:(c + 1) * 128, fg * 512:(fg + 1) * 512], 512)
                        for j in range(4):
                            mm(banks2[j].t[:, 0:CAP], wt.t[:, j * 128:(j + 1) * 128], x_T.t[:, c, :], c == 0, c == 15, [wt.b, x_T.b], [banks2[j].b])
                    for j in range(4):
                        f = fg * 4 + j
                        op("vector", lambda e: e.tensor_tensor(out=a_T.t[:, f, :], in0=banks2[j].t[:, 0:CAP], in1=silus[j].t[:, :], op=ALU.mult),
                           [banks2[j].b, silus[j].b], [a_T.b])
                idf = id_.t.bitcast(F32)
                for cg in range(4):
                    banks = [psn() for _ in range(CB)]
                    for f in range(8):
                        wt = wload(w_ed[ex, f * 128:(f + 1) * 128, cg * 512:(cg + 1) * 512], 512)
                        for bl in range(CB):
                            mm(banks[bl].t[:, :], a_T.t[:, f, bl * 128:(bl + 1) * 128], wt.t[:, :], f == 0, f == 7, [wt.b, a_T.b], [banks[bl].b])
                    for bl in range(CB):
                        y_ = ysb[yi % 4]; yi += 1
                        eng = ev_eng()
                        if eng == "vector":
                            op("vector", lambda e: e.tensor_scalar(out=y_.t[:, 0:512], in0=banks[bl].t[:, :], scalar1=idf[:, bl, 1:2],
                                                                   scalar2=None, op0=ALU.mult), [banks[bl].b, id_.b], [y_.b])
                        else:
                            op("scalar", lambda e: e.activation(out=y_.t[:, 0:512], in_=banks[bl].t[:, :], func=AF.Copy, scale=idf[:, bl, 1:2]),
                               [banks[bl].b, id_.b], [y_.b])
                        r0 = ex * CAP + bl * 128
                        dma("sync", lambda e: e.dma_start(out=ys_d[r0:r0 + 128, cg * 512:(cg + 1) * 512], in_=y_.t[:, 0:512]),
                            reads=[y_.b], writes=[])
            Sx.barrier()

        with ExitStack() as pe:
          if stop >= 6:
            gfin = sbt(pe, "gfin", [128, D], F32)
            dma("sync", lambda e: e.dma_start(out=gfin.t[:], in_=g_final.partition_broadcast(128)), writes=[gfin.b])
            x2 = [sbt(pe, "x2_%d" % i, [128, D], F32) for i in range(2)]
            ya = [sbt(pe, "ya%d" % i, [128, D], F32) for i in range(2)]
            yb = [sbt(pe, "yb%d" % i, [128, D], F32) for i in range(2)]
            junk = sbt(pe, "junkE", [128, D], BF16)
            ss = [sbt(pe, "ssE%d" % i, [128, 1], F32) for i in range(2)]
            yo = [sbt(pe, "yo%d" % i, [128, D], F32) for i in range(2)]
            outb = Buf()
            for ob in range(NOB):
                i = ob % 2
                row = ob * 128
                dma("sync", lambda e: e.dma_start(out=x2[i].t[:], in_=x2_d[row:row + 128, :]), reads=[b_x2[ob]], writes=[x2[i].b])
                dma("gpsimd", lambda e: e.indirect_dma_start(out=ya[i].t[:, :], out_offset=None, in_=ys_d[:, :],
                                                             in_offset=bass.IndirectOffsetOnAxis(ap=slots.t[:, ob, 0:1], axis=0),
                                                             bounds_check=reg_nslot, oob_is_err=False),
                    reads=[slots.b], writes=[ya[i].b])
                dma("gpsimd", lambda e: e.indirect_dma_start(out=yb[i].t[:, :], out_offset=None, in_=ys_d[:, :],
                                                             in_offset=bass.IndirectOffsetOnAxis(ap=slots.t[:, ob, 1:2], axis=0),
                                                             bounds_check=reg_nslot, oob_is_err=False),
                    reads=[slots.b], writes=[yb[i].b])
                op("vector", lambda e: e.tensor_tensor(out=x2[i].t[:], in0=x2[i].t[:], in1=ya[i].t[:], op=ALU.add),
                   [x2[i].b, ya[i].b], [x2[i].b])
                op("gpsimd", lambda e: e.tensor_tensor(out=x2[i].t[:], in0=x2[i].t[:], in1=yb[i].t[:], op=ALU.add),
                   [x2[i].b, yb[i].b], [x2[i].b])
                rms_rstd(None, x2[i].t[:], x2[i].b, D, ss[i], junk)
                op("vector", lambda e: e.scalar_tensor_tensor(out=yo[i].t[:], in0=x2[i].t[:], scalar=ss[i].t[:, 0:1], in1=gfin.t[:],
                                                              op0=ALU.mult, op1=ALU.mult), [x2[i].b, ss[i].b, gfin.b], [yo[i].b])
                dma("sync", lambda e: e.dma_start(out=yout[row:row + 128, :], in_=yo[i].t[:]), reads=[yo[i].b], writes=[outb])
            Sx.barrier()
        print("built: ninst=%d nsem=%d" % (Sx.ninst, Sx.nsem))
    return nc


_CACHE = {}


def _diag_masks(r):
    s = np.arange(128)[:, None]
    t = np.arange(128)[None, :]
    strict = (s < t).astype(np.float32)
    m = np.zeros((4, 128, 128), np.float32)
    for k in range(4):
        if k < r:
            m[k] = 1.0
        elif k == r:
            m[k] = strict
    return m


def run(inputs, S, CB=3, debug=False, trace=False, stop=6):
    key = (S, CB, debug, stop)
    if key not in _CACHE:
        _CACHE[key] = build(S, CB, debug, stop)
    nc = _CACHE[key]
    x = np.asarray(inputs["x"], np.float32)
    mem = np.asarray(inputs["mem"], np.float32)
    NB = S // 128
    shared = {}
    for k_ in ("g_mix", "w_in", "g_gm", "w_spatial", "b_spatial", "w_branch_sb", "w_branch_gm", "w_out", "g_cross", "g_mem",
               "w_xq", "w_xkv", "w_xo", "g_ffn", "w_rg", "b_rg", "w_re", "b_re", "w_e_gate", "w_e_up", "w_e_down"):
        if stop < 5 and k_.startswith("w_e_"):
            continue
        shared[k_] = np.ascontiguousarray(np.asarray(inputs[k_], np.float32)[0])
    shared["g_final"] = np.ascontiguousarray(np.asarray(inputs["g_final"], np.float32))
    in_maps = []
    for c in range(8):
        b, r = c // 4, c % 4
        xbatch = np.ascontiguousarray(x[b])
        own = np.ascontiguousarray(xbatch.reshape(NB // 4, 4, 128, D)[:, r].reshape(-1, D))
        d = dict(shared)
        d["xb"] = xbatch
        d["xown"] = own
        d["memb"] = np.ascontiguousarray(mem[b])
        d["maskd"] = _diag_masks(r)
        in_maps.append(d)
    res = run_bass_kernel_spmd(nc, in_maps, core_ids=list(range(8)), trace=trace)
    out = np.empty((2, S, D), np.float32)
    for c in range(8):
        b, r = c // 4, c % 4
        y = np.asarray(res.results[c]["yout"]).reshape(NB // 4, 128, D)
        out[b].reshape(NB // 4, 4, 128, D)[:, r] = y
    return out, res


def kernel(**inputs):
    S = np.asarray(inputs["x"]).shape[1]
    out, _ = run(inputs, S)
    return out
```
